# Optimizing a Trainium2 kernel written in Bass

```python
import math
import jax
import jax.numpy as jnp
from jax import lax
import numpy as np

D_MODEL = 1024
BATCH = 32
SEQ = 2048
DEPTH = 2

GRID_W = 64
CTX_LEN = 256
EPS = 1e-6
N_MOD = 6

CONV_W = 512
CONV_K = 3

RG_W = 512
RG_BLOCKS = 8
RG_BLOCK_W = RG_W // RG_BLOCKS
RG_CONV_K = 4
RG_C = 8.0

DA_HEADS = 4
DA_HEAD_DIM = 64
DA_V_DIM = 2 * DA_HEAD_DIM
DA_QK_W = DA_HEADS * 2 * DA_HEAD_DIM
DA_W = DA_HEADS * DA_V_DIM
Q_BLOCK = 128
ROPE_AXIS_DIM = DA_HEAD_DIM // 2
ROPE_BASE = 10000.0

N_BRANCH = 3
SPLIT_SIZES = (CONV_W, CONV_W, CONV_W, RG_W, RG_W, DA_QK_W, DA_QK_W, DA_W, D_MODEL, D_MODEL, D_MODEL)
D_IN = 3 * CONV_W + 2 * RG_W + 2 * DA_QK_W + DA_W + N_BRANCH * D_MODEL

FF_DENSE = 3584
N_EXPERTS = 8
TOP_K = 2
FF_EXPERT = 3584
MOE_BLOCK = 512
N_DENSE = (DEPTH + 1) // 2
N_MOE = DEPTH // 2

kernel_name = 'hybrid_flow_block'


def rmsnorm(x, w):
    xf = x.astype(jnp.float32)
    y = xf * lax.rsqrt(jnp.mean(xf * xf, axis=-1, keepdims=True) + EPS)
    return (y * w.astype(jnp.float32)).astype(x.dtype)


def modulate(h, shift, scale):
    return h * (1.0 + scale) + shift


def split_columns(z):
    out, start = [], 0
    for size in SPLIT_SIZES:
        out.append(z[..., start:start + size])
        start += size
    return out


def dwconv(x, w, pad_left, pad_right):
    n = x.shape[1]
    xp = jnp.pad(x, ((0, 0), (pad_left, pad_right), (0, 0)))
    return sum(xp[:, k:k + n] * w[k] for k in range(w.shape[0]))


def short_conv_mixer(gate_b, gate_c, xin, w):
    return gate_b * dwconv(gate_c * xin, w, CONV_K // 2, CONV_K // 2)


def _linrec_combine(e1, e2):
    a1, b1 = e1
    a2, b2 = e2
    return a1 * a2, a2 * b1 + b2


def rglru_direction(xr, h0, conv_w, conv_b, wa, ba, wx, bx, lam, reverse):
    pad = (0, RG_CONV_K - 1) if reverse else (RG_CONV_K - 1, 0)
    xc = dwconv(xr, conv_w, pad[0], pad[1]) + conv_b
    bsz, n, _ = xc.shape
    xg = xc.reshape(bsz, n, RG_BLOCKS, RG_BLOCK_W)
    r = jax.nn.sigmoid(jnp.einsum('bngi,gij->bngj', xg, wa) + ba).reshape(bsz, n, RG_W)
    i = jax.nn.sigmoid(jnp.einsum('bngi,gij->bngj', xg, wx) + bx).reshape(bsz, n, RG_W)
    log_a = (-RG_C * r.astype(jnp.float32)) * jax.nn.softplus(-lam.astype(jnp.float32))
    a = jnp.exp(log_a)
    b = jnp.sqrt(-jnp.expm1(2.0 * log_a)) * (i * xc).astype(jnp.float32)
    if h0 is not None:
        t0 = n - 1 if reverse else 0
        b = b.at[:, t0].add(a[:, t0] * h0)
    _, h = lax.associative_scan(_linrec_combine, (a, b), reverse=reverse, axis=1)
    return h


def axial_rope_tables(n, dtype):
    rows = n // GRID_W
    pos_r = jnp.repeat(jnp.arange(rows, dtype=jnp.float32), GRID_W)
    pos_c = jnp.broadcast_to(jnp.arange(GRID_W, dtype=jnp.float32), (rows, GRID_W)).reshape(-1)
    inv_freq = ROPE_BASE ** (-jnp.arange(0, ROPE_AXIS_DIM, 2, dtype=jnp.float32) / ROPE_AXIS_DIM)
    ang = jnp.stack([pos_r[:, None] * inv_freq, pos_c[:, None] * inv_freq], axis=1)
    return jnp.cos(ang).astype(dtype), jnp.sin(ang).astype(dtype)


def apply_axial_rope(x, cos, sin):
    shp = x.shape
    xa = x.reshape(shp[:-1] + (2, 2, ROPE_AXIS_DIM // 2))
    x1, x2 = xa[..., 0, :], xa[..., 1, :]
    cb = cos[None, :, None, None]
    sb = sin[None, :, None, None]
    out = jnp.stack([x1 * cb - x2 * sb, x2 * cb + x1 * sb], axis=-2)
    return out.reshape(shp)


def diff_lambda(da_lambda, lam_init):
    lf = da_lambda.astype(jnp.float32)
    return jnp.exp(jnp.sum(lf[0] * lf[1])) - jnp.exp(jnp.sum(lf[2] * lf[3])) + lam_init


def diff_softmax_mix(q1, q2, k1, k2, v, lam):
    scale = DA_HEAD_DIM ** -0.5
    p1 = jax.nn.softmax(jnp.einsum('bqhd,bkhd->bhqk', q1, k1).astype(jnp.float32) * scale, axis=-1)
    p2 = jax.nn.softmax(jnp.einsum('bqhd,bkhd->bhqk', q2, k2).astype(jnp.float32) * scale, axis=-1)
    p = (p1 - lam * p2).astype(v.dtype)
    return jnp.einsum('bhqk,bkhe->bqhe', p, v)


def latent_diff_attention(q, k, v, ck, cv, lam):
    bsz, n = q.shape[:2]
    kk = jnp.concatenate([k, ck], axis=1)
    vv = jnp.concatenate([v, cv], axis=1)
    k1, k2 = kk[..., 0, :], kk[..., 1, :]
    nb = n // Q_BLOCK
    qb = q.reshape(bsz, nb, Q_BLOCK, DA_HEADS, 2, DA_HEAD_DIM).swapaxes(0, 1)
    o = lax.map(lambda qblk: diff_softmax_mix(qblk[..., 0, :], qblk[..., 1, :], k1, k2, vv, lam), qb)
    return o.swapaxes(0, 1).reshape(bsz, n, DA_HEADS, DA_V_DIM)


def diff_attn_out(o, subln_w, lam_init):
    o = rmsnorm(o, subln_w) * (1.0 - lam_init)
    return o.reshape(o.shape[:2] + (DA_W,))


def gated_merge(y_a, y_r, y_d, g_a, g_r, g_d, w_branch, w_out):
    m = (jax.nn.sigmoid(g_a) * (y_a @ w_branch[0])
         + jax.nn.sigmoid(g_r) * (y_r @ w_branch[1])
         + jax.nn.sigmoid(g_d) * (y_d @ w_branch[2]))
    return m @ w_out


def hybrid_mixer(u, uc, cos, sin, w_in, conv_a_w, rg_conv_w, rg_conv_b, rg_wa, rg_ba, rg_wx, rg_bx,
                 rg_lambda, da_lambda, da_subln_w, w_branch, w_out, lam_init, need_ctx):
    bsz, n, _ = u.shape
    n_ctx = uc.shape[1]
    a_b, a_c, a_x, r_g, r_x, q, k, v, g_a, g_r, g_d = split_columns(u @ w_in)
    ca_b, ca_c, ca_x, cr_g, cr_x, cq, ck, cv, cg_a, cg_r, cg_d = split_columns(uc @ w_in)

    fwd = (rg_conv_w[0], rg_conv_b[0], rg_wa[0], rg_ba[0], rg_wx[0], rg_bx[0], rg_lambda[0])
    bwd = (rg_conv_w[1], rg_conv_b[1], rg_wa[1], rg_ba[1], rg_wx[1], rg_bx[1], rg_lambda[1])
    hf_ctx = rglru_direction(cr_x, None, *fwd, reverse=False)
    hb_ctx = rglru_direction(cr_x, None, *bwd, reverse=True)
    hf = rglru_direction(r_x, hf_ctx[:, -1], *fwd, reverse=False)
    hb = rglru_direction(r_x, hb_ctx[:, 0], *bwd, reverse=True)
    y_r = jax.nn.gelu(r_g) * (hf + hb).astype(r_g.dtype)

    lam = diff_lambda(da_lambda, lam_init)
    q = apply_axial_rope(q.reshape(bsz, n, DA_HEADS, 2, DA_HEAD_DIM), cos, sin)
    k = apply_axial_rope(k.reshape(bsz, n, DA_HEADS, 2, DA_HEAD_DIM), cos, sin)
    v = v.reshape(bsz, n, DA_HEADS, DA_V_DIM)
    ck = ck.reshape(bsz, n_ctx, DA_HEADS, 2, DA_HEAD_DIM)
    cv = cv.reshape(bsz, n_ctx, DA_HEADS, DA_V_DIM)
    y_d = diff_attn_out(latent_diff_attention(q, k, v, ck, cv, lam), da_subln_w, lam_init)

    y_a = short_conv_mixer(a_b, a_c, a_x, conv_a_w)
    y = gated_merge(y_a, y_r, y_d, g_a, g_r, g_d, w_branch, w_out)
    if not need_ctx:
        return y, None

    yc_a = short_conv_mixer(ca_b, ca_c, ca_x, conv_a_w)
    yc_r = jax.nn.gelu(cr_g) * (hf_ctx + hb_ctx).astype(cr_g.dtype)
    cq = cq.reshape(bsz, n_ctx, DA_HEADS, 2, DA_HEAD_DIM)
    oc = diff_softmax_mix(cq[..., 0, :], cq[..., 1, :], ck[..., 0, :], ck[..., 1, :], cv, lam)
    yc_d = diff_attn_out(oc, da_subln_w, lam_init)
    yc = gated_merge(yc_a, yc_r, yc_d, cg_a, cg_r, cg_d, w_branch, w_out)
    return y, yc


def swiglu(h, w13, w2):
    g, u = jnp.split(h @ w13, 2, axis=-1)
    return (jax.nn.silu(g) * u) @ w2


def moe_swiglu(h, router_w, w13, w2):
    shp = h.shape
    t = h.reshape(-1, D_MODEL)
    n_tok = t.shape[0]
    logits = (t @ router_w).astype(jnp.float32)
    top_v, top_e = lax.top_k(logits, TOP_K)
    gates = jax.nn.softmax(top_v, axis=-1)
    flat_e = top_e.reshape(-1)
    flat_tok = jnp.repeat(jnp.arange(n_tok, dtype=jnp.int32), TOP_K)
    flat_g = gates.reshape(-1)
    order = jnp.argsort(flat_e)
    se, stok, sg = flat_e[order], flat_tok[order], flat_g[order]
    counts = jnp.bincount(flat_e, length=N_EXPERTS)
    padded = (counts + MOE_BLOCK - 1) // MOE_BLOCK * MOE_BLOCK
    pad_end = jnp.cumsum(padded)
    pad_start = pad_end - padded
    start = jnp.cumsum(counts) - counts
    n_slots = n_tok * TOP_K
    dest = pad_start[se] + jnp.arange(n_slots, dtype=pad_start.dtype) - start[se]
    n_blocks = -(-n_slots // MOE_BLOCK) + N_EXPERTS
    buf_tok = jnp.full((n_blocks * MOE_BLOCK,), n_tok, dtype=jnp.int32).at[dest].set(stok)
    block_e = jnp.minimum(jnp.searchsorted(pad_end, jnp.arange(n_blocks) * MOE_BLOCK, side='right'),
                          N_EXPERTS - 1)
    t_pad = jnp.concatenate([t, jnp.zeros((1, D_MODEL), t.dtype)], axis=0)
    xb = t_pad[buf_tok].reshape(n_blocks, MOE_BLOCK, D_MODEL)
    yb = lax.map(lambda a: swiglu(a[0], w13[a[1]], w2[a[1]]), (xb, block_e))
    ys = yb.reshape(-1, D_MODEL)[dest]
    out = jnp.zeros_like(t).at[stok].add(sg[:, None].astype(t.dtype) * ys)
    return out.reshape(shp)


def setup_inputs(seed: int = 0) -> dict:
    key = jax.random.key(seed)
    keys = jax.random.split(key, 32)
    f32 = jnp.float32

    def normal(i, shape, scale=1.0):
        return jax.random.normal(keys[i], shape, f32) * scale

    u = jax.random.uniform(keys[20], (DEPTH, 2, RG_W), f32, 0.9, 0.999)
    a0 = u ** (1.0 / RG_C)
    return {
        'x': normal(0, (BATCH, SEQ, D_MODEL)),
        'c': normal(1, (BATCH, D_MODEL)),
        'ctx': normal(2, (BATCH, CTX_LEN, D_MODEL)),
        'c_ctx': normal(3, (D_MODEL,)),
        'mod_w': normal(4, (DEPTH, D_MODEL, N_MOD * D_MODEL), D_MODEL ** -0.5),
        'mod_b': normal(5, (DEPTH, N_MOD * D_MODEL), 0.02),
        'norm1_w': 1.0 + normal(6, (DEPTH, D_MODEL), 0.02),
        'norm2_w': 1.0 + normal(7, (DEPTH, D_MODEL), 0.02),
        'w_in': normal(8, (DEPTH, D_MODEL, D_IN), D_MODEL ** -0.5),
        'conv_a_w': normal(9, (DEPTH, CONV_K, CONV_W), CONV_K ** -0.5),
        'rg_conv_w': normal(10, (DEPTH, 2, RG_CONV_K, RG_W), RG_CONV_K ** -0.5),
        'rg_conv_b': normal(11, (DEPTH, 2, RG_W), 0.02),
        'rg_wa': normal(12, (DEPTH, 2, RG_BLOCKS, RG_BLOCK_W, RG_BLOCK_W), RG_BLOCK_W ** -0.5),
        'rg_ba': normal(13, (DEPTH, 2, RG_BLOCKS, RG_BLOCK_W), 0.02),
        'rg_wx': normal(14, (DEPTH, 2, RG_BLOCKS, RG_BLOCK_W, RG_BLOCK_W), RG_BLOCK_W ** -0.5),
        'rg_bx': normal(15, (DEPTH, 2, RG_BLOCKS, RG_BLOCK_W), 0.02),
        'rg_lambda': jnp.log(a0) - jnp.log1p(-a0),
        'da_lambda': normal(16, (DEPTH, 4, DA_HEAD_DIM), 0.1),
        'da_subln_w': 1.0 + normal(17, (DEPTH, DA_V_DIM), 0.02),
        'w_branch': normal(18, (DEPTH, N_BRANCH, CONV_W, D_MODEL), CONV_W ** -0.5),
        'w_out': normal(19, (DEPTH, D_MODEL, D_MODEL), D_MODEL ** -0.5),
        'ffn_w13': normal(21, (N_DENSE, D_MODEL, 2 * FF_DENSE), D_MODEL ** -0.5),
        'ffn_w2': normal(22, (N_DENSE, FF_DENSE, D_MODEL), FF_DENSE ** -0.5),
        'router_w': normal(23, (N_MOE, D_MODEL, N_EXPERTS), D_MODEL ** -0.5),
        'moe_w13': normal(24, (N_MOE, N_EXPERTS, D_MODEL, 2 * FF_EXPERT), D_MODEL ** -0.5),
        'moe_w2': normal(25, (N_MOE, N_EXPERTS, FF_EXPERT, D_MODEL), FF_EXPERT ** -0.5),
        'final_norm_w': 1.0 + normal(26, (D_MODEL,), 0.02),
    }


def reference(x, c, ctx, c_ctx, mod_w, mod_b, norm1_w, norm2_w, w_in, conv_a_w, rg_conv_w, rg_conv_b,
              rg_wa, rg_ba, rg_wx, rg_bx, rg_lambda, da_lambda, da_subln_w, w_branch, w_out,
              ffn_w13, ffn_w2, router_w, moe_w13, moe_w2, final_norm_w):
    n = x.shape[1]
    cos, sin = axial_rope_tables(n, x.dtype)
    cx = ctx
    for l in range(DEPTH):
        last = l == DEPTH - 1
        lam_init = 0.8 - 0.6 * math.exp(-0.3 * l)
        mod = jax.nn.silu(c) @ mod_w[l] + mod_b[l]
        mod_c = jax.nn.silu(c_ctx) @ mod_w[l] + mod_b[l]
        sh1, sc1, g1, sh2, sc2, g2 = jnp.split(mod[:, None, :], N_MOD, axis=-1)
        csh1, csc1, cg1, csh2, csc2, cg2 = jnp.split(mod_c, N_MOD)

        u = modulate(rmsnorm(x, norm1_w[l]), sh1, sc1)
        uc = modulate(rmsnorm(cx, norm1_w[l]), csh1, csc1)
        y, yc = hybrid_mixer(u, uc, cos, sin, w_in[l], conv_a_w[l], rg_conv_w[l], rg_conv_b[l],
                             rg_wa[l], rg_ba[l], rg_wx[l], rg_bx[l], rg_lambda[l], da_lambda[l],
                             da_subln_w[l], w_branch[l], w_out[l], lam_init, not last)
        x = x + g1 * y
        if not last:
            cx = cx + cg1 * yc

        j = l // 2
        h = modulate(rmsnorm(x, norm2_w[l]), sh2, sc2)
        if l % 2 == 0:
            x = x + g2 * swiglu(h, ffn_w13[j], ffn_w2[j])
        else:
            x = x + g2 * moe_swiglu(h, router_w[j], moe_w13[j], moe_w2[j])
        if not last:
            hc = modulate(rmsnorm(cx, norm2_w[l]), csh2, csc2)
            if l % 2 == 0:
                cx = cx + cg2 * swiglu(hc, ffn_w13[j], ffn_w2[j])
            else:
                cx = cx + cg2 * moe_swiglu(hc, router_w[j], moe_w13[j], moe_w2[j])
    return rmsnorm(x, final_norm_w)
```

```python
import contextlib
import math
import numpy as np
import concourse.bass as bass
import concourse.mybir as mybir
from concourse.bass_utils import run_bass_kernel_spmd

F32 = mybir.dt.float32
BF16 = mybir.dt.bfloat16
AF = mybir.ActivationFunctionType
ALU = mybir.AluOpType
AX = mybir.AxisListType

D = 1024
SEQ = 2048
CTX = 256
DEPTH = 2
NCORE = 8
FF = 3584
NEXP = 8
D_IN = 7168
EPS = 1e-6
C_AB, C_AC, C_AX, C_RG, C_RX, C_Q, C_K, C_V, C_GA, C_GR, C_GD = (
    0, 512, 1024, 1536, 2048, 2560, 3072, 3584, 4096, 5120, 6144)


class Buf:
    __slots__ = ("name", "w", "r", "excl")

    def __init__(self, name="", excl=False):
        self.name = name
        self.w = None
        self.r = {}
        self.excl = excl


class Sched:
    ENG = ("pe", "act", "dve", "pool", "sp")
    NSLOT = 12

    def __init__(self, nc):
        self.nc = nc
        self.streams = {e: [] for e in self.ENG}
        self.cnt = {}
        self.seen = {e: {} for e in self.ENG}
        self.sems = {}
        self.slot_rr = {"sp": 0, "pool": 0}

    def alloc_sems(self, stack):
        for e in self.ENG:
            self.sems[e] = stack.enter_context(self.nc.semaphore("s_" + e))
            self.cnt[e] = 0
        for q in ("sp", "pool"):
            for i in range(self.NSLOT):
                k = "%s_d%d" % (q, i)
                self.sems[k] = stack.enter_context(self.nc.semaphore("s_" + k))
                self.cnt[k] = 0

    def _wait(self, eng, key, val):
        if val <= 0 or self.seen[eng].get(key, 0) >= val:
            return
        self.seen[eng][key] = val
        self.streams[eng].append(("wait", key, val))

    def _deps(self, eng, reads, writes):
        for b in reads:
            if b.w is not None and not (eng == "pe" and b.w[0] == "pe"):
                self._wait(eng, b.w[0], b.w[1])
            if b.excl:
                for k, v in b.r.items():
                    if k != eng:
                        self._wait(eng, k, v)
        for b in writes:
            if b.w is not None and not (eng == "pe" and b.w[0] == "pe"):
                self._wait(eng, b.w[0], b.w[1])
            for k, v in b.r.items():
                if not (eng == "pe" and k == "pe"):
                    self._wait(eng, k, v)

    def _mark(self, tok, reads, writes):
        for b in reads:
            b.r[tok[0]] = tok[1]
        for b in writes:
            b.w = tok
            b.r = {}

    def op(self, eng, fn, reads=(), writes=()):
        self._deps(eng, reads, writes)
        self.cnt[eng] += 1
        self.streams[eng].append(("op", fn, eng, 1))
        self._mark((eng, self.cnt[eng]), reads, writes)

    def dma(self, q, fn, reads=(), writes=()):
        self._deps(q, reads, writes)
        s = self.slot_rr[q]
        self.slot_rr[q] = (s + 1) % self.NSLOT
        key = "%s_d%d" % (q, s)
        self._wait(q, key, self.cnt[key])
        self.cnt[key] += 16
        self.streams[q].append(("op", fn, key, 16))
        self._mark((key, self.cnt[key]), reads, writes)

    def barrier(self):
        for e in self.ENG:
            for key, v in self.cnt.items():
                self._wait(e, key, v)

    def finish(self, eng="sp"):
        for key, v in self.cnt.items():
            self._wait(eng, key, v)

    def replay(self, block):
        sems = self.sems

        def run(name, handle):
            for it in self.streams[name]:
                if it[0] == "wait":
                    handle.wait_ge(sems[it[1]], it[2])
                else:
                    it[1](handle).then_inc(sems[it[2]], it[3])

        @block.tensor
        def _(e):
            run("pe", e)

        @block.scalar
        def _(e):
            run("act", e)

        @block.vector
        def _(e):
            run("dve", e)

        @block.gpsimd
        def _(e):
            run("pool", e)

        @block.sync
        def _(e):
            run("sp", e)


def seq(fns):
    def f(e):
        r = None
        for g in fns:
            r = g(e)
        return r
    return f


class _Stop(Exception):
    pass


def build_nc(NB, n_layers=DEPTH, stop=None):
    nc = bass.Bass("TRN2", target_bir_lowering=False)
    ckn = [0]

    def ck(tag=""):
        ckn[0] += 1
        if stop is not None and ckn[0] >= stop:
            print("STOP at checkpoint", ckn[0], tag, flush=True)
            raise _Stop()

    def din(name, shape):
        return nc.dram_tensor(name, list(shape), F32, kind="ExternalInput").ap()

    x_d = din("x", (NB, SEQ, D))
    ctx_d = din("ctx", (NB, CTX, D))
    cc_d = din("cc", (NB + 1, D))
    mod_w = din("mod_w", (DEPTH, D, 6 * D))
    mod_b = din("mod_b", (DEPTH, 6 * D))
    norm1_w = din("norm1_w", (DEPTH, D))
    norm2_w = din("norm2_w", (DEPTH, D))
    w_in = din("w_in", (DEPTH, D, D_IN))
    conv_a_w = din("conv_a_w", (DEPTH, 3, 512))
    rg_conv_w = din("rg_conv_w", (DEPTH, 2, 4, 512))
    rg_conv_b = din("rg_conv_b", (DEPTH, 2, 512))
    rg_wa = din("rg_wa", (DEPTH, 2, 8, 64, 64))
    rg_ba = din("rg_ba", (DEPTH, 2, 8, 64))
    rg_wx = din("rg_wx", (DEPTH, 2, 8, 64, 64))
    rg_bx = din("rg_bx", (DEPTH, 2, 8, 64))
    rg_lambda = din("rg_lambda", (DEPTH, 2, 512))
    da_lambda = din("da_lambda", (DEPTH, 4, 64))
    da_subln_w = din("da_subln_w", (DEPTH, 128))
    w_branch = din("w_branch", (DEPTH, 3, 512, D))
    w_out = din("w_out", (DEPTH, D, D))
    ffn_w13 = din("ffn_w13", (1, D, 2 * FF))
    ffn_w2 = din("ffn_w2", (1, FF, D))
    router_w = din("router_w", (1, D, NEXP))
    moe_w13 = din("moe_w13", (1, NEXP, D, 2 * FF))
    moe_w2 = din("moe_w2", (1, NEXP, FF, D))
    final_norm_w = din("final_norm_w", (D,))
    ident_d = din("ident", (128, 128))
    pmat_d = din("pmat", (128, 128))
    rope_d = din("rope", (2, 128, SEQ))
    out_d = nc.dram_tensor("out", [NB, SEQ, D], F32, kind="ExternalOutput").ap()
    kscr = nc.dram_tensor("kscr", [DEPTH, 4, 128, CTX], BF16, kind="Internal").ap()
    vscr = nc.dram_tensor("vscr", [DEPTH, 4, 128, 2, 128], BF16, kind="Internal").ap()

    S = Sched(nc)
    with contextlib.ExitStack() as st:
        S.alloc_sems(st)

        def sb(name, shape, dt):
            return st.enter_context(nc.sbuf_tensor("sb_" + name, list(shape), dt))

        xT = sb("xT", (128, 8, SEQ), F32)
        uT = sb("uT", (128, 8, SEQ), BF16)
        Y = sb("Y", (128, 24576), BF16)
        ft = [sb("ft%d" % i, (128, 512), F32) for i in range(5)]
        bt = [sb("bt%d" % i, (128, 512), BF16) for i in range(3)]
        PR = sb("PR", (128, 4224), BF16)
        wbuf = [sb("wb%d" % i, (128, 8, 128), BF16) for i in range(1)]
        NWB = 1
        hcar = sb("hcar", (128, 2), F32)
        ident = sb("ident", (128, 128), F32)
        onesb = sb("onesb", (128, 128), BF16)
        pmat = sb("pmat", (128, 128), BF16)
        rgW = sb("rgW", (128, 4, 128), BF16)
        prow = ft[1][:, 0:128]
        pcolA = sb("pcolA", (128, DEPTH, 96), F32)
        pcolB = sb("pcolB", (128, 112), F32)
        ccol = sb("ccol", (128, 8 * (NB + 1)), F32)
        scol = sb("scol", (128, 8, NB + 1), BF16)
        modc = sb("modc", (128, DEPTH, NB + 1, 48), F32)
        wm = sb("wm", (128, 16), F32)
        lamt = ft[2][:, 0:256]
        lamc = sb("lamc", (128, DEPTH, 4), F32)
        cst = sb("cst", (128, 4), F32)
        Kc = sb("Kc", (128, CTX), BF16)
        Vc = sb("Vc", (128, 2, 128), BF16)
        hsave = sb("hsave", (128, DEPTH, 4, 2), F32)
        rt = sb("rt", (128, 48), F32)
        ps = [st.enter_context(nc.psum_tensor("ps%d" % i, [128, 512], F32)) for i in range(8)]
        PB = [Buf("ps%d" % i, excl=True) for i in range(8)]
        _rem = nc.sbuf_bytes_remaining
        assert 212863 - _rem <= 179000, ("SBUF over the safe limit", 212863 - _rem)

        yA = Y[:, 0:8192].rearrange("p (j t) -> p j t", j=4)
        yR = Y[:, 8192:16384].rearrange("p (j t) -> p j t", j=4)
        yD = Y[:, 16384:24576].rearrange("p (j t) -> p j t", j=4)
        ropec = Y[:, 0:2048]
        ropes = Y[:, 2048:4096]
        qT = Y[:, 4096:6144]
        kT = Y[:, 6144:8192]
        vtok = Y[:, 8192:10240].rearrange("p (c d) -> p c d", d=128)
        qT2 = Y[:, 10240:12288]
        def ffw(i):
            base = i * 12288
            return (Y[:, base:base + 4096].rearrange("p (k n) -> p k n", k=8),
                    Y[:, base + 4096:base + 8192].rearrange("p (k n) -> p k n", k=8),
                    Y[:, base + 8192:base + 12288].rearrange("p (f n) -> p f n", f=4))
        def modst(i):
            return Y[:, i * 4096:(i + 1) * 4096].rearrange("p (k n) -> p k n", k=8)
        iost = Y[:, 16384:24576].bitcast(F32).rearrange("p (t d) -> p t d", t=4)
        WTt = sb("WTt", (128, 16, 8), F32)

        B = {}

        def gb(name):
            if name not in B:
                B[name] = Buf(name)
            return B[name]

        bank_rr = [0]

        def bank(lo=0, hi=8):
            i = lo + bank_rr[0] % (hi - lo)
            bank_rr[0] += 1
            return i

        wb_rr = [0]

        def ACT(out, in_, func, reads, writes, scale=None, bias=None):
            kw = {}
            if scale is not None:
                kw["scale"] = scale
            if bias is not None:
                kw["bias"] = bias
            S.op("act", lambda e: e.activation(out=out, in_=in_, func=func, **kw), reads, writes)

        def TT(out, a, b, op, reads, writes):
            S.op("dve", lambda e: e.tensor_tensor(out=out, in0=a, in1=b, op=op), reads, writes)

        def STT(out, in0, scalar, in1, op0, op1, reads, writes):
            S.op("dve", lambda e: e.scalar_tensor_tensor(out=out, in0=in0, scalar=scalar, in1=in1, op0=op0, op1=op1), reads, writes)

        def TS(out, in0, s1, s2, op0, op1, reads, writes):
            if s2 is None:
                S.op("dve", lambda e: e.tensor_scalar(out=out, in0=in0, scalar1=s1, scalar2=None, op0=op0), reads, writes)
            else:
                S.op("dve", lambda e: e.tensor_scalar(out=out, in0=in0, scalar1=s1, scalar2=s2, op0=op0, op1=op1), reads, writes)

        def RECIP(out, in_, reads, writes):
            S.op("dve", lambda e: e.reciprocal(out=out, in_=in_), reads, writes)

        def COPYV(out, in_, reads, writes):
            S.op("dve", lambda e: e.tensor_copy(out=out, in_=in_), reads, writes)

        def MM(items, reads, writes):
            fns = [(lambda e, o=o, l=l, r=r, a=a, z=z: e.matmul(o, l, r, start=a, stop=z)) for (o, l, r, a, z) in items]
            S.op("pe", seq(fns), reads, writes)

        def TR(out, in_, reads, writes):
            S.op("pe", lambda e: e.transpose(out, in_, ident[:]), list(reads) + [gb("ident")], writes)

        def DMAW(out, in_, reads, writes):
            S.dma("pool", lambda e: e.dma_start(out=out, in_=in_), reads, writes)

        def DMAS(out, in_, reads, writes):
            S.dma("sp", lambda e: e.dma_start(out=out, in_=in_), reads, writes)

        DMAS(ident[:], ident_d[:, :], [], [gb("ident")])
        DMAW(pmat[:], pmat_d[:, :], [], [gb("pmat")])
        S.op("dve", lambda e: e.memset(onesb[:], 1.0), [], [gb("onesb")])
        S.op("dve", lambda e: e.memset(cst[:, 0:1], EPS), [], [gb("cst")])
        S.op("dve", lambda e: e.memset(cst[:, 1:2], 1.0), [], [gb("cst")])
        S.op("dve", lambda e: e.memset(cst[:, 2:3], 0.0), [], [gb("cst")])
        S.op("dve", lambda e: e.memset(rgW[:], 0.0), [], [gb("rgW")])
        S.op("dve", lambda e: e.memset(prow, 0.0), [], [gb("prow")])

        def rows_to_cols(row_specs, nrows, dst):
            for ap, r0, r in row_specs:
                DMAS(prow[r0:r0 + r, :], ap, [], [gb("prow")])
            pb = bank()
            TR(ps[pb][:, 0:128], prow, [gb("prow")], [PB[pb]])
            COPYV(dst, ps[pb][:, 0:nrows], [PB[pb]], [gb("pcol")])

        for l in range(DEPTH):
            specs = [
                (norm1_w[l].rearrange("(r p) -> r p", p=128), 0, 8),
                (norm2_w[l].rearrange("(r p) -> r p", p=128), 8, 8),
                (conv_a_w[l].rearrange("k (j p) -> (k j) p", p=128), 16, 12),
                (rg_conv_w[l].rearrange("d k (j p) -> (d k j) p", p=128), 28, 32),
                (rg_conv_b[l].rearrange("d (j p) -> (d j) p", p=128), 60, 8),
                (rg_ba[l].rearrange("d (j g) i -> (d j) (g i)", g=2), 68, 8),
                (rg_bx[l].rearrange("d (j g) i -> (d j) (g i)", g=2), 76, 8),
                (rg_lambda[l].rearrange("d (j p) -> (d j) p", p=128), 84, 8),
            ]
            rows_to_cols(specs, 92, pcolA[:, l, 0:92])
            ACT(pcolA[:, l, 84:92], pcolA[:, l, 84:92], AF.Exp, [gb("pcol")], [gb("pcol")], scale=-1.0)
            ACT(pcolA[:, l, 84:92], pcolA[:, l, 84:92], AF.Ln, [gb("pcol")], [gb("pcol")], scale=1.0, bias=cst[:, 1:2])
            TS(pcolA[:, l, 84:92], pcolA[:, l, 84:92], -8.0, None, ALU.mult, None, [gb("pcol")], [gb("pcol")])
        specs = [
            (mod_b[0].rearrange("(r p) -> r p", p=128), 0, 48),
            (mod_b[1].rearrange("(r p) -> r p", p=128), 48, 48),
            (final_norm_w.rearrange("(r p) -> r p", p=128), 96, 8),
            (da_subln_w[0].rearrange("(r p) -> r p", p=128), 104, 1),
            (da_subln_w[1].rearrange("(r p) -> r p", p=128), 105, 1),
        ]
        rows_to_cols(specs, 106, pcolB[:, 0:106])
        lam_init = [0.8 - 0.6 * math.exp(-0.3 * l) for l in range(DEPTH)]
        for l in range(DEPTH):
            TS(pcolB[:, 104 + l:105 + l], pcolB[:, 104 + l:105 + l], 1.0 - lam_init[l], None, ALU.mult, None, [gb("pcol")], [gb("pcol")])
        rows_to_cols([(cc_d.rearrange("b (r p) -> (b r) p", p=128), 0, 8 * (NB + 1))], 8 * (NB + 1), ccol[:, :])
        ACT(scol[:].rearrange("p k b -> p b k"), ccol[:].rearrange("p (b k) -> p b k", k=8), AF.Silu, [gb("pcol")], [gb("scol")])
        for l in range(DEPTH):
            DMAS(lamt, da_lambda[l].rearrange("a d -> (a d)").partition_broadcast(128), [], [gb("lamt")])
            TT(lamt[:, 0:64], lamt[:, 0:64], lamt[:, 64:128], ALU.mult, [gb("lamt")], [gb("lamt")])
            TT(lamt[:, 128:192], lamt[:, 128:192], lamt[:, 192:256], ALU.mult, [gb("lamt")], [gb("lamt")])
            S.op("dve", lambda e, l=l: e.reduce_sum(out=lamc[:, l, 1:2], in_=lamt[:, 0:64], axis=AX.X), [gb("lamt")], [gb("lamc")])
            S.op("dve", lambda e, l=l: e.reduce_sum(out=lamc[:, l, 2:3], in_=lamt[:, 128:192], axis=AX.X), [gb("lamt")], [gb("lamc")])
            ACT(lamc[:, l, 1:3], lamc[:, l, 1:3], AF.Exp, [gb("lamc")], [gb("lamc")])
            STT(lamc[:, l, 0:1], lamc[:, l, 2:3], -lam_init[l], lamc[:, l, 1:2], ALU.add, ALU.subtract, [gb("lamc")], [gb("lamc")])

        for l in range(DEPTH):
            pbm = bank()
            for g in range(12):
                stg = modst(g % 2)
                sbuf_b = gb("modst%d" % (g % 2))
                DMAW(stg, mod_w[l][:, g * 512:(g + 1) * 512].rearrange("(k p) n -> p k n", p=128), [], [sbuf_b])
                items = []
                for oc in range(4):
                    occ = g * 4 + oc
                    for kk in range(8):
                        items.append((ps[pbm][:, occ * (NB + 1):(occ + 1) * (NB + 1)],
                                      stg[:, kk, oc * 128:(oc + 1) * 128], scol[:, kk, :], kk == 0, kk == 7))
                MM(items, [sbuf_b, gb("scol")], [PB[pbm]])
            for r in range(NB + 1):
                TT(modc[:, l, r, :], ps[pbm][:, 0:48 * (NB + 1)].rearrange("p (o r) -> p r o", r=NB + 1)[:, r, :],
                   pcolB[:, l * 48:(l + 1) * 48], ALU.add, [PB[pbm], gb("pcol")], [gb("modc")])
        S.barrier()
        try:
            ck("setup")
        except _Stop:
            stop = -1

        def load_w(src_cols_ap):
            i = wb_rr[0] % NWB
            wb_rr[0] += 1
            DMAW(wbuf[i][:], src_cols_ap.rearrange("(k p) n -> p k n", p=128), [], [gb("wb%d" % i)])
            return wbuf[i], gb("wb%d" % i)

        def proj(w, wB, rhs_k, n, reads, lo=0, hi=8):
            pb = bank(lo, hi)
            MM([(ps[pb][:, 0:n], w[:, kk, :], rhs_k(kk), kk == 0, kk == 7) for kk in range(8)],
               [wB] + list(reads), [PB[pb]])
            return pb

        def norm_mod(T, blocks, N, col_w, sh_ap, dst_bf, XB, UB, f32_tap=None):
            for bi in blocks:
                t0 = bi * N
                pb = bank()
                items = []
                for c in range(8):
                    sq = bt[c % 2]
                    sqB = gb("bt%d" % (c % 2))
                    ACT(sq[:, 0:N], xT[:, c, t0:t0 + N], AF.Square, [XB[bi]], [sqB])
                    MM([(ps[pb][:, 0:N], onesb[:], sq[:, 0:N], c == 0, c == 7)], [sqB, gb("onesb")], [PB[pb]])
                rstd = ft[4]
                ACT(rstd[:, 0:N], ps[pb][:, 0:N], AF.Sqrt, [PB[pb], gb("cst")], [gb("ft4")], scale=1.0 / D, bias=cst[:, 0:1])
                RECIP(rstd[:, 0:N], rstd[:, 0:N], [gb("ft4")], [gb("ft4")])
                for c in range(8):
                    tmp = ft[c % 2]
                    tB = gb("ft%d" % (c % 2))
                    TT(tmp[:, 0:N], xT[:, c, t0:t0 + N], rstd[:, 0:N], ALU.mult, [XB[bi], gb("ft4")], [tB])
                    if f32_tap is not None:
                        ACT(tmp[:, 0:N], tmp[:, 0:N], AF.Identity, [tB, gb("wm"), gb("modc")], [tB],
                            scale=col_w[:, c:c + 1], bias=sh_ap[:, c:c + 1])
                        f32_tap(bi, c, tmp, tB)
                        COPYV(dst_bf[:, c, t0:t0 + N], tmp[:, 0:N], [tB], [UB[bi]])
                    else:
                        ACT(dst_bf[:, c, t0:t0 + N], tmp[:, 0:N], AF.Identity, [tB, gb("wm"), gb("modc")], [UB[bi]],
                            scale=col_w[:, c:c + 1], bias=sh_ap[:, c:c + 1])

        def run_pass(b, is_ctx):
            T = CTX if is_ctx else SEQ
            N = min(512, T)
            nblk = T // N
            ntt = N // 128
            r = NB if is_ctx else b
            XB = [gb("X%d" % i) for i in range(nblk)]
            UB = [gb("U%d" % i) for i in range(nblk)]
            YAB = [gb("YA%d" % i) for i in range(nblk)]
            YRB = [gb("YR%d" % i) for i in range(nblk)]
            YDB = [gb("YD%d" % i) for i in range(nblk)]
            src = ctx_d[b] if is_ctx else x_d[b]

            for bi in range(nblk):
                DMAS(iost[:, 0:ntt, :], src[bi * N:(bi + 1) * N, :].rearrange("(t p) d -> p t d", p=128), [], [gb("iost")])
                for c in range(8):
                    pb = bank()
                    for tt in range(ntt):
                        TR(ps[pb][:, tt * 128:(tt + 1) * 128], iost[:, tt, c * 128:(c + 1) * 128], [gb("iost")], [PB[pb]])
                    if c % 2 == 0:
                        ACT(xT[:, c, bi * N:(bi + 1) * N], ps[pb][:, 0:N], AF.Copy, [PB[pb]], [XB[bi]])
                    else:
                        COPYV(xT[:, c, bi * N:(bi + 1) * N], ps[pb][:, 0:N], [PB[pb]], [XB[bi]])

            ck("load")
            for l in range(n_layers):
                partial = is_ctx and (l == DEPTH - 1)
                pc = pcolA[:, l, :]
                mc = modc[:, l, r, :]
                STT(wm[:, 0:8], mc[:, 8:16], 1.0, pc[:, 0:8], ALU.add, ALU.mult, [gb("modc"), gb("pcol")], [gb("wm")])
                STT(wm[:, 8:16], mc[:, 32:40], 1.0, pc[:, 8:16], ALU.add, ALU.mult, [gb("modc"), gb("pcol")], [gb("wm")])
                norm_mod(T, range(nblk), N, wm[:, 0:8], mc[:, 0:8], uT, XB, UB)

                ck("norm1")

                def u_k(bi):
                    return lambda kk: uT[:, kk, bi * N:(bi + 1) * N]


                ck("rgW")
                if not is_ctx:
                    DMAW(ropec, rope_d[0], [], [gb("rope")])
                    DMAW(ropes, rope_d[1], [], [gb("rope")])
                S.op("dve", lambda e, T=T: e.memset(qT[64:128, 0:T], 0.0), [], [gb("qT")])
                S.op("dve", lambda e, T=T: e.memset(qT2[0:64, 0:T], 0.0), [], [gb("qT")])
                for h in range(4):
                    for which, c0, dstT, dB in ((0, C_K, kT, gb("kT")), (1, C_Q, qT, gb("qT"))):
                        if partial and which == 1:
                            continue
                        w, wB = load_w(w_in[l][:, c0 + h * 128:c0 + (h + 1) * 128])
                        if h == 0 and which == 0:
                            ck("kload")
                        for bi in range(nblk):
                            pb = proj(w, wB, u_k(bi), N, [UB[bi]], 0, 4)
                            sl = slice(bi * N, (bi + 1) * N)
                            if h == 0 and which == 0 and bi == 0:
                                ck("kproj")
                            if is_ctx:
                                if which == 0:
                                    ACT(dstT[:, sl], ps[pb][:, 0:N], AF.Copy, [PB[pb]], [dB])
                                    if h == 0:
                                        ck("kact")
                                    DMAS(kscr[l, h], dstT[:, sl], [dB], [gb("kscr")])
                                    if h == 0:
                                        ck("ksave")
                                else:
                                    ACT(qT[0:64, sl], ps[pb][0:64, 0:N], AF.Copy, [PB[pb]], [dB])
                                    ACT(qT2[64:128, sl], ps[pb][64:128, 0:N], AF.Copy, [PB[pb]], [dB])
                            else:
                                raw = bt[2]
                                rB = gb("bt2")
                                ACT(raw[:, 0:N], ps[pb][:, 0:N], AF.Copy, [PB[pb]], [rB])
                                pb2 = bank(0, 4)
                                MM([(ps[pb2][:, 0:N], pmat[:], raw[:, 0:N], True, True)], [rB, gb("pmat")], [PB[pb2]])
                                if h == 0 and which == 0:
                                    ck("ropemm%d" % bi)
                                t1 = ft[2]
                                t2 = ft[3]
                                ACT(t1[:, 0:N], ps[pb][:, 0:N], AF.Copy, [PB[pb]], [gb("ft2")])
                                TT(t1[:, 0:N], t1[:, 0:N], ropec[:, sl], ALU.mult, [gb("ft2"), gb("rope")], [gb("ft2")])
                                if h == 0 and which == 0:
                                    ck("ropet1%d" % bi)
                                ACT(t2[:, 0:N], ps[pb2][:, 0:N], AF.Copy, [PB[pb2]], [gb("ft3")])
                                TT(t2[:, 0:N], t2[:, 0:N], ropes[:, sl], ALU.mult, [gb("ft3"), gb("rope")], [gb("ft3")])
                                if which == 0:
                                    TT(dstT[:, sl], t1[:, 0:N], t2[:, 0:N], ALU.add, [gb("ft2"), gb("ft3")], [dB])
                                else:
                                    TT(qT[0:64, sl], t1[0:64, 0:N], t2[0:64, 0:N], ALU.add, [gb("ft2"), gb("ft3")], [dB])
                                    TT(qT2[64:128, sl], t1[64:128, 0:N], t2[64:128, 0:N], ALU.add, [gb("ft2"), gb("ft3")], [dB])
                            if h == 0 and which == 0 and not is_ctx:
                                ck("kblk%d" % bi)
                        if h == 0 and which == 0 and not is_ctx:
                            ck("kdone")
                    if h == 0:
                        ck("kq")
                    w, wB = load_w(w_in[l][:, C_V + h * 128:C_V + (h + 1) * 128])
                    for tg in range(T // 512 if T >= 512 else 1):
                        pb = bank(0, 4)
                        nt = min(4, T // 128)
                        items = []
                        for tt in range(nt):
                            tok0 = (tg * 4 + tt) * 128
                            for kk in range(8):
                                items.append((ps[pb][:, tt * 128:(tt + 1) * 128], uT[:, kk, tok0:tok0 + 128], w[:, kk, :], kk == 0, kk == 7))
                        MM(items, [wB] + UB, [PB[pb]])
                        ACT(vtok[:, tg * 4:tg * 4 + nt, :], ps[pb][:, 0:nt * 128].rearrange("p (t d) -> p t d", d=128), AF.Copy, [PB[pb]], [gb("vtok")])
                        if is_ctx:
                            DMAS(vscr[l, h], vtok[:, 0:2, :], [gb("vtok")], [gb("vscr")])
                    if h == 0:
                        ck("v")
                    if partial:
                        continue
                    keys = []
                    if not is_ctx:
                        DMAS(Kc[:], kscr[l, h], [gb("kscr")], [gb("Kc")])
                        DMAS(Vc[:], vscr[l, h], [gb("vscr")], [gb("Vc")])
                        for kc in range(2):
                            keys.append((Kc[:, kc * 128:(kc + 1) * 128], Vc[:, kc, :], [gb("Kc"), gb("Vc")]))
                    for kc in range(T // 128):
                        keys.append((kT[:, kc * 128:(kc + 1) * 128], vtok[:, kc, :], [gb("kT"), gb("vtok")]))
                    nk = len(keys)
                    for bi in range(nblk):
                        sl = slice(bi * N, (bi + 1) * N)
                        for ki, (kap, vap, kB) in enumerate(keys):
                            for m in range(2):
                                pbs = m * 2 + (ki % 2)
                                qsrc = qT if m == 0 else qT2
                                MM([(ps[pbs][:, 0:N], kap, qsrc[:, sl], True, True)], kB + [gb("qT")], [PB[pbs]])
                                P = bt[m]
                                PBf = gb("bt%d" % m)
                                ACT(P[:, 0:N], ps[pbs][:, 0:N], AF.Exp, [PB[pbs]], [PBf], scale=0.125)
                                MM([(ps[4 + 2 * m][:, 0:N], vap, P[:, 0:N], ki == 0, ki == nk - 1),
                                    (ps[5 + 2 * m][:, 0:N], onesb[:], P[:, 0:N], ki == 0, ki == nk - 1)],
                                   kB + [PBf, gb("onesb")], [PB[4 + 2 * m], PB[5 + 2 * m]])
                        if h == 0 and bi == 0:
                            ck("scores")
                        RECIP(ft[0][:, 0:N], ps[5][:, 0:N], [PB[5]], [gb("ft0")])
                        TT(ft[0][:, 0:N], ps[4][:, 0:N], ft[0][:, 0:N], ALU.mult, [PB[4], gb("ft0")], [gb("ft0")])
                        RECIP(ft[1][:, 0:N], ps[7][:, 0:N], [PB[7]], [gb("ft1")])
                        TT(ft[1][:, 0:N], ps[6][:, 0:N], ft[1][:, 0:N], ALU.mult, [PB[6], gb("ft1")], [gb("ft1")])
                        STT(ft[0][:, 0:N], ft[1][:, 0:N], lamc[:, l, 0:1], ft[0][:, 0:N], ALU.mult, ALU.add, [gb("ft1"), gb("ft0"), gb("lamc")], [gb("ft0")])
                        ACT(bt[2][:, 0:N], ft[0][:, 0:N], AF.Square, [gb("ft0")], [gb("bt2")])
                        MM([(ps[0][:, 0:N], onesb[:], bt[2][:, 0:N], True, True)], [gb("bt2"), gb("onesb")], [PB[0]])
                        ACT(ft[1][:, 0:N], ps[0][:, 0:N], AF.Sqrt, [PB[0], gb("cst")], [gb("ft1")], scale=1.0 / 128, bias=cst[:, 0:1])
                        RECIP(ft[1][:, 0:N], ft[1][:, 0:N], [gb("ft1")], [gb("ft1")])
                        TT(ft[0][:, 0:N], ft[0][:, 0:N], ft[1][:, 0:N], ALU.mult, [gb("ft0"), gb("ft1")], [gb("ft0")])
                        ACT(yD[:, h, sl], ft[0][:, 0:N], AF.Identity, [gb("ft0"), gb("pcol")], [YDB[bi]], scale=pcolB[:, 104 + l:105 + l])
                S.barrier()
                ck("attn")

                if not partial:
                    cx = PR[:, 0:T + 2]
                    S.op("dve", lambda e: e.memset(PR[:, 0:1], 0.0), [], [gb("cx")])
                    S.op("dve", lambda e, T=T: e.memset(PR[:, T + 1:T + 2], 0.0), [], [gb("cx")])
                    for j in range(4):
                        acst = PR[:, 2112:2112 + T]
                        wc, wcB = load_w(w_in[l][:, C_AC + j * 128:C_AC + (j + 1) * 128])
                        for bi in range(nblk):
                            pbc = proj(wc, wcB, u_k(bi), N, [UB[bi]])
                            ACT(acst[:, bi * N:(bi + 1) * N], ps[pbc][:, 0:N], AF.Copy, [PB[pbc]], [gb("acst")])
                        wx_, wxB = load_w(w_in[l][:, C_AX + j * 128:C_AX + (j + 1) * 128])
                        for bi in range(nblk):
                            pbx = proj(wx_, wxB, u_k(bi), N, [UB[bi]])
                            TT(PR[:, 1 + bi * N:1 + (bi + 1) * N], ps[pbx][:, 0:N], acst[:, bi * N:(bi + 1) * N], ALU.mult, [PB[pbx], gb("acst")], [gb("cx")])
                        wb_, wbB = load_w(w_in[l][:, C_AB + j * 128:C_AB + (j + 1) * 128])
                        for bi in range(nblk):
                            pbb = proj(wb_, wbB, u_k(bi), N, [UB[bi]])
                            t = ft[1]
                            o = bi * N
                            TS(t[:, 0:N], PR[:, o:o + N], pc[:, 16 + j:17 + j], None, ALU.mult, None, [gb("cx"), gb("pcol")], [gb("ft1")])
                            STT(t[:, 0:N], PR[:, o + 1:o + 1 + N], pc[:, 20 + j:21 + j], t[:, 0:N], ALU.mult, ALU.add, [gb("cx"), gb("pcol"), gb("ft1")], [gb("ft1")])
                            STT(t[:, 0:N], PR[:, o + 2:o + 2 + N], pc[:, 24 + j:25 + j], t[:, 0:N], ALU.mult, ALU.add, [gb("cx"), gb("pcol"), gb("ft1")], [gb("ft1")])
                            TT(yA[:, j, o:o + N], ps[pbb][:, 0:N], t[:, 0:N], ALU.mult, [PB[pbb], gb("ft1")], [YAB[bi]])
                    S.barrier()
                    ck("mixA")

                xr = PR[:, 0:T + 6]
                hF = PR[:, 2112:2112 + T]
                for j in range(4):
                    S.op("dve", lambda e: e.memset(PR[:, 0:3], 0.0), [], [gb("xr")])
                    S.op("dve", lambda e, T=T: e.memset(PR[:, T + 3:T + 6], 0.0), [], [gb("xr")])
                    for which, wsrc in enumerate((rg_wa, rg_wx)):
                        for d in range(2):
                            for g in range(2):
                                DMAW(rgW[g * 64:(g + 1) * 64, which * 2 + d, g * 64:(g + 1) * 64], wsrc[l, d, 2 * j + g], [], [gb("rgW")])
                    w, wB = load_w(w_in[l][:, C_RX + j * 128:C_RX + (j + 1) * 128])
                    for bi in range(nblk):
                        pb = proj(w, wB, u_k(bi), N, [UB[bi]])
                        ACT(PR[:, 3 + bi * N:3 + (bi + 1) * N], ps[pb][:, 0:N], AF.Copy, [PB[pb]], [gb("xr")])
                    if not partial:
                        wg, wgB = load_w(w_in[l][:, C_RG + j * 128:C_RG + (j + 1) * 128])
                    for d in range(2):
                        order = list(range(nblk)) if d == 0 else list(range(nblk - 1, -1, -1))
                        prev_h = None
                        for oi, bi in enumerate(order):
                            o = bi * N + (0 if d == 0 else 3)
                            xc = bt[2]
                            cw = 28 + d * 16
                            TS(ft[0][:, 0:N], PR[:, o:o + N], pc[:, cw + j:cw + j + 1], pc[:, 60 + d * 4 + j:61 + d * 4 + j], ALU.mult, ALU.add, [gb("xr"), gb("pcol")], [gb("ft0")])
                            for k in range(1, 4):
                                dst = xc[:, 0:N] if k == 3 else ft[0][:, 0:N]
                                dB = gb("bt2") if k == 3 else gb("ft0")
                                STT(dst, PR[:, o + k:o + k + N], pc[:, cw + 4 * k + j:cw + 4 * k + j + 1], ft[0][:, 0:N], ALU.mult, ALU.add, [gb("xr"), gb("pcol"), gb("ft0")], [dB])
                            pa = bank()
                            pi = bank()
                            MM([(ps[pa][:, 0:N], rgW[:, d, :], xc[:, 0:N], True, True)], [gb("rgW"), gb("bt2")], [PB[pa]])
                            MM([(ps[pi][:, 0:N], rgW[:, 2 + d, :], xc[:, 0:N], True, True)], [gb("rgW"), gb("bt2")], [PB[pi]])
                            ACT(ft[1][:, 0:N], ps[pa][:, 0:N], AF.Sigmoid, [PB[pa], gb("pcol")], [gb("ft1")], scale=1.0, bias=pc[:, 68 + d * 4 + j:69 + d * 4 + j])
                            ACT(ft[2][:, 0:N], ps[pi][:, 0:N], AF.Sigmoid, [PB[pi], gb("pcol")], [gb("ft2")], scale=1.0, bias=pc[:, 76 + d * 4 + j:77 + d * 4 + j])
                            ACT(ft[1][:, 0:N], ft[1][:, 0:N], AF.Exp, [gb("ft1"), gb("pcol")], [gb("ft1")], scale=pc[:, 84 + d * 4 + j:85 + d * 4 + j])
                            ACT(ft[3][:, 0:N], ft[1][:, 0:N], AF.Square, [gb("ft1")], [gb("ft3")])
                            ACT(ft[3][:, 0:N], ft[3][:, 0:N], AF.Sqrt, [gb("ft3"), gb("cst")], [gb("ft3")], scale=-1.0, bias=cst[:, 1:2])
                            TT(ft[2][:, 0:N], ft[2][:, 0:N], xc[:, 0:N], ALU.mult, [gb("ft2"), gb("bt2")], [gb("ft2")])
                            TT(ft[2][:, 0:N], ft[2][:, 0:N], ft[3][:, 0:N], ALU.mult, [gb("ft2"), gb("ft3")], [gb("ft2")])
                            hcur = ft[4]
                            hB = gb("ft4")
                            if prev_h is None:
                                init = cst[:, 2:3] if is_ctx else hsave[:, l, j, d:d + 1]
                                iB = [gb("cst"), gb("hsave")]
                            else:
                                COPYV(hcar[:, d:d + 1], hcur[:, N - 1:N] if d == 0 else hcur[:, 0:1], [hB], [gb("hcar")])
                                init = hcar[:, d:d + 1]
                                iB = [gb("hcar")]
                            if d == 0:
                                S.op("dve", lambda e, hcur=hcur, init=init, N=N: e.tensor_tensor_scan(out=hcur[:, 0:N], data0=ft[1][:, 0:N], data1=ft[2][:, 0:N], initial=init, op0=ALU.mult, op1=ALU.add),
                                     [gb("ft1"), gb("ft2")] + iB, [hB])
                                if not partial:
                                    ACT(hF[:, bi * N:(bi + 1) * N], hcur[:, 0:N], AF.Copy, [hB], [gb("hF")])
                            else:
                                S.op("dve", lambda e, hcur=hcur, init=init, N=N: e.tensor_tensor_scan(out=hcur[:, N - 1::-1] if False else hcur[:, 0:N][:, ::-1], data0=ft[1][:, 0:N][:, ::-1], data1=ft[2][:, 0:N][:, ::-1], initial=init, op0=ALU.mult, op1=ALU.add),
                                     [gb("ft1"), gb("ft2")] + iB, [hB])
                                if not partial:
                                    pg = proj(wg, wgB, u_k(bi), N, [UB[bi]])
                                    ACT(ft[3][:, 0:N], ps[pg][:, 0:N], AF.Gelu_apprx_tanh, [PB[pg]], [gb("ft3")])
                                    TT(ft[0][:, 0:N], hcur[:, 0:N], hF[:, bi * N:(bi + 1) * N], ALU.add, [hB, gb("hF")], [gb("ft0")])
                                    TT(yR[:, j, bi * N:(bi + 1) * N], ft[0][:, 0:N], ft[3][:, 0:N], ALU.mult, [gb("ft0"), gb("ft3")], [YRB[bi]])
                            prev_h = (hcur, hB)
                        if is_ctx:
                            last = prev_h[0][:, N - 1:N] if d == 0 else prev_h[0][:, 0:1]
                            COPYV(hsave[:, l, j, d:d + 1], last, [prev_h[1]], [gb("hsave")])
                S.barrier()
                ck("mixB")
                if partial:
                    continue

                mT = PR[:, 0:4096].rearrange("p (c n) -> p c n", c=8)
                for bi in range(nblk):
                    sl = slice(bi * N, (bi + 1) * N)
                    for c in range(8):
                        for br, (c0, ysrc, yB) in enumerate(((C_GA, yA, YAB), (C_GR, yR, YRB), (C_GD, yD, YDB))):
                            w, wB = load_w(w_in[l][:, c0 + c * 128:c0 + (c + 1) * 128])
                            pg = proj(w, wB, u_k(bi), N, [UB[bi]])
                            i = wb_rr[0] % NWB
                            wb_rr[0] += 1
                            DMAW(wbuf[i][:, 0:4, :], w_branch[l, br][:, c * 128:(c + 1) * 128].rearrange("(k p) n -> p k n", p=128), [], [gb("wb%d" % i)])
                            pbr = bank()
                            MM([(ps[pbr][:, 0:N], wbuf[i][:, kk, :], ysrc[:, kk, sl], kk == 0, kk == 3) for kk in range(4)],
                               [gb("wb%d" % i), yB[bi]], [PB[pbr]])
                            sg = ft[br % 2]
                            sgB = gb("ft%d" % (br % 2))
                            ACT(sg[:, 0:N], ps[pg][:, 0:N], AF.Sigmoid, [PB[pg]], [sgB])
                            if br == 0:
                                TT(ft[2][:, 0:N], ps[pbr][:, 0:N], sg[:, 0:N], ALU.mult, [PB[pbr], sgB], [gb("ft2")])
                            else:
                                TT(ft[3][:, 0:N], ps[pbr][:, 0:N], sg[:, 0:N], ALU.mult, [PB[pbr], sgB], [gb("ft3")])
                                dst = mT[:, c, 0:N] if br == 2 else ft[2][:, 0:N]
                                dB = gb("mT") if br == 2 else gb("ft2")
                                TT(dst, ft[2][:, 0:N], ft[3][:, 0:N], ALU.add, [gb("ft2"), gb("ft3")], [dB])
                    for c2 in range(8):
                        w, wB = load_w(w_out[l][:, c2 * 128:(c2 + 1) * 128])
                        py = proj(w, wB, lambda kk: mT[:, kk, 0:N], N, [gb("mT")])
                        STT(xT[:, c2, sl], ps[py][:, 0:N], mc[:, 16 + c2:17 + c2], xT[:, c2, sl], ALU.mult, ALU.add, [PB[py], gb("modc"), XB[bi]], [XB[bi]])
                S.barrier()
                ck("merge")

                is_moe = (l % 2 == 1)
                hT = uT
                if is_moe:
                    rw = ft[3]
                    DMAS(rw[:, 0:64].rearrange("p (k e) -> p k e", e=8), router_w[0].rearrange("(k p) e -> p k e", p=128), [], [gb("ft3")])
                    WT = WTt[:]
                    pr_bank = [None]

                    def tap(bi, c, tmp, tB):
                        if c == 0:
                            pr_bank[0] = [bank() for _ in range(ntt)]
                        pbs_ = pr_bank[0]
                        MM([(ps[pbs_[tt]][:, 0:8], tmp[:, tt * 128:(tt + 1) * 128], rw[:, c * 8:(c + 1) * 8], c == 0, c == 7) for tt in range(ntt)],
                           [tB, gb("ft3")], [PB[p_] for p_ in pbs_])
                        if c == 7:
                            for tt in range(ntt):
                                gt = bi * ntt + tt
                                L = rt[:, 0:8]
                                pb = pbs_[tt]
                                COPYV(L, ps[pb][:, 0:8], [PB[pb]], [gb("rt")])
                                S.op("dve", lambda e: e.reduce_max(out=rt[:, 40:41], in_=rt[:, 0:8], axis=AX.X), [gb("rt")], [gb("rt")])
                                TS(rt[:, 8:16], rt[:, 0:8], rt[:, 40:41], None, ALU.is_equal, None, [gb("rt")], [gb("rt")])
                                STT(rt[:, 16:24], rt[:, 8:16], -1e30, rt[:, 0:8], ALU.mult, ALU.add, [gb("rt")], [gb("rt")])
                                S.op("dve", lambda e: e.reduce_max(out=rt[:, 41:42], in_=rt[:, 16:24], axis=AX.X), [gb("rt")], [gb("rt")])
                                TS(rt[:, 24:32], rt[:, 16:24], rt[:, 41:42], None, ALU.is_equal, None, [gb("rt")], [gb("rt")])
                                TT(rt[:, 42:43], rt[:, 41:42], rt[:, 40:41], ALU.subtract, [gb("rt")], [gb("rt")])
                                ACT(rt[:, 42:43], rt[:, 42:43], AF.Exp, [gb("rt")], [gb("rt")])
                                TS(rt[:, 43:44], rt[:, 42:43], 1.0, None, ALU.add, None, [gb("rt")], [gb("rt")])
                                RECIP(rt[:, 43:44], rt[:, 43:44], [gb("rt")], [gb("rt")])
                                TT(rt[:, 44:45], rt[:, 42:43], rt[:, 43:44], ALU.mult, [gb("rt")], [gb("rt")])
                                TS(rt[:, 32:40], rt[:, 8:16], rt[:, 43:44], None, ALU.mult, None, [gb("rt")], [gb("rt")])
                                STT(WT[:, gt, :], rt[:, 24:32], rt[:, 44:45], rt[:, 32:40], ALU.mult, ALU.add, [gb("rt")], [gb("WT")])
                    norm_mod(T, range(nblk), N, wm[:, 8:16], mc[:, 24:32], hT, XB, UB, f32_tap=tap)
                else:
                    norm_mod(T, range(nblk), N, wm[:, 8:16], mc[:, 24:32], hT, XB, UB)

                act = [PR[:, i * 2048:(i + 1) * 2048].rearrange("p (f n) -> p f n", f=4) for i in range(2)]
                experts = range(NEXP) if is_moe else [None]
                grp = 0
                for ex in experts:
                    w13 = moe_w13[0, ex] if is_moe else ffn_w13[0]
                    w2 = moe_w2[0, ex] if is_moe else ffn_w2[0]
                    for gi in range(7):
                        Wg, Wu, W2 = ffw(grp % 2)
                        fB = gb("ffw%d" % (grp % 2))
                        grp += 1
                        DMAW(Wg, w13[:, gi * 512:(gi + 1) * 512].rearrange("(k p) n -> p k n", p=128), [], [fB])
                        DMAW(Wu, w13[:, FF + gi * 512:FF + (gi + 1) * 512].rearrange("(k p) n -> p k n", p=128), [], [fB])
                        DMAW(W2, w2[gi * 512:(gi + 1) * 512, :].rearrange("(f p) n -> p f n", p=128), [], [fB])
                        for bi in range(nblk):
                            sl = slice(bi * N, (bi + 1) * N)
                            a_t = act[bi % 2]
                            aB = gb("act%d" % (bi % 2))
                            if is_moe:
                                pgb = bank()
                                for tt in range(ntt):
                                    gt = bi * ntt + tt
                                    TS(ft[4][:, tt * 128:(tt + 1) * 128], onesb[:], WT[:, gt, ex:ex + 1], None, ALU.mult, None, [gb("onesb"), gb("WT")], [gb("ft4")])
                                MM([(ps[pgb][:, tt * 128:(tt + 1) * 128], ft[4][:, tt * 128:(tt + 1) * 128], ident[:], True, True) for tt in range(ntt)],
                                   [gb("ft4"), gb("ident")], [PB[pgb]])
                                ACT(ft[3][:, 0:N], ps[pgb][:, 0:N], AF.Copy, [PB[pgb]], [gb("ft3")])
                            for f in range(4):
                                pg = bank()
                                MM([(ps[pg][:, 0:N], Wg[:, kk, f * 128:(f + 1) * 128], hT[:, kk, sl], kk == 0, kk == 7) for kk in range(8)], [fB, UB[bi]], [PB[pg]])
                                pu = bank()
                                MM([(ps[pu][:, 0:N], Wu[:, kk, f * 128:(f + 1) * 128], hT[:, kk, sl], kk == 0, kk == 7) for kk in range(8)], [fB, UB[bi]], [PB[pu]])
                                sg = bt[f % 2]
                                sgB = gb("bt%d" % (f % 2))
                                ACT(sg[:, 0:N], ps[pg][:, 0:N], AF.Silu, [PB[pg]], [sgB])
                                if is_moe:
                                    tmpf = ft[f % 2]
                                    tfB = gb("ft%d" % (f % 2))
                                    TT(tmpf[:, 0:N], ps[pu][:, 0:N], sg[:, 0:N], ALU.mult, [PB[pu], sgB], [tfB])
                                    TT(a_t[:, f, 0:N], tmpf[:, 0:N], ft[3][:, 0:N], ALU.mult, [tfB, gb("ft3")], [aB])
                                else:
                                    TT(a_t[:, f, 0:N], ps[pu][:, 0:N], sg[:, 0:N], ALU.mult, [PB[pu], sgB], [aB])
                            for c2 in range(8):
                                py = bank()
                                MM([(ps[py][:, 0:N], W2[:, f, c2 * 128:(c2 + 1) * 128], a_t[:, f, 0:N], f == 0, f == 3) for f in range(4)], [fB, aB], [PB[py]])
                                STT(xT[:, c2, sl], ps[py][:, 0:N], mc[:, 40 + c2:41 + c2], xT[:, c2, sl], ALU.mult, ALU.add, [PB[py], gb("modc"), XB[bi]], [XB[bi]])
                S.barrier()
                ck("ffn")

            if is_ctx:
                return
            for bi in range(nblk):
                t0 = bi * N
                pb = bank()
                for c in range(8):
                    sq = bt[c % 2]
                    sqB = gb("bt%d" % (c % 2))
                    ACT(sq[:, 0:N], xT[:, c, t0:t0 + N], AF.Square, [XB[bi]], [sqB])
                    MM([(ps[pb][:, 0:N], onesb[:], sq[:, 0:N], c == 0, c == 7)], [sqB, gb("onesb")], [PB[pb]])
                rstd = ft[4]
                ACT(rstd[:, 0:N], ps[pb][:, 0:N], AF.Sqrt, [PB[pb], gb("cst")], [gb("ft4")], scale=1.0 / D, bias=cst[:, 0:1])
                RECIP(rstd[:, 0:N], rstd[:, 0:N], [gb("ft4")], [gb("ft4")])
                for c in range(8):
                    tmp = ft[c % 2]
                    tB = gb("ft%d" % (c % 2))
                    STT(tmp[:, 0:N], xT[:, c, t0:t0 + N], pcolB[:, 96 + c:97 + c], rstd[:, 0:N], ALU.mult, ALU.mult, [XB[bi], gb("ft4"), gb("pcol")], [tB])
                    pt = bank()
                    for tt in range(ntt):
                        TR(ps[pt][:, tt * 128:(tt + 1) * 128], tmp[:, tt * 128:(tt + 1) * 128], [tB], [PB[pt]])
                    if c % 2 == 0:
                        ACT(iost[:, 0:ntt, c * 128:(c + 1) * 128], ps[pt][:, 0:N].rearrange("p (t d) -> p t d", d=128), AF.Copy, [PB[pt]], [gb("iost")])
                    else:
                        COPYV(iost[:, 0:ntt, c * 128:(c + 1) * 128], ps[pt][:, 0:N].rearrange("p (t d) -> p t d", d=128), [PB[pt]], [gb("iost")])
                DMAS(out_d[b, t0:t0 + N, :].rearrange("(t p) d -> p t d", p=128), iost[:, 0:ntt, :], [gb("iost")], [gb("outd")])
            S.barrier()

        try:
            if stop != -1:
                for b in range(NB):
                    run_pass(b, True)
                    run_pass(b, False)
        except _Stop:
            S.barrier()
            dbg = []
            for c in range(8):
                dbg.append((uT[:, c, 0:256], c, 256))
                dbg.append((xT[:, c, 0:256], 20 + c, 256))
                dbg.append((xT[:, c, 1792:2048], 40 + c, 256))
            for j in range(4):
                dbg.append((yD[:, j, 0:256], 8 + j, 256))
                dbg.append((yA[:, j, 0:256], 12 + j, 256))
                dbg.append((yR[:, j, 0:256], 16 + j, 256))
                dbg.append((yD[:, j, 1792:2048], 32 + j, 256))
                dbg.append((yR[:, j, 1792:2048], 36 + j, 256))
                dbg.append((yA[:, j, 1792:2048], 53 + j, 256))
            dbg += [(kT[:, 0:256], 28, 256), (qT[:, 0:256], 29, 256), (qT2[:, 0:256], 30, 256),
                    (vtok[:, 0:2, :], 31, 256),
                    (modc[:].rearrange("p l r o -> p (l r o)"), 48, DEPTH * (NB + 1) * 48), (wm[:], 49, 16),
                    (pcolA[:].rearrange("p l o -> p (l o)"), 50, 192), (pcolB[:], 51, 112),
                    (hsave[:].rearrange("p l j d -> p (l j d)"), 52, 16), (WTt[:].rearrange("p t e -> p (t e)"), 57, 128),
                    (lamc[:].rearrange("p l o -> p (l o)"), 58, 8)]
            for ap, k, n in dbg:
                r0 = (k // 4) * 128
                c0 = (k % 4) * 256
                if len(ap.shape) == 3:
                    COPYV(ft[0][:, 0:n].rearrange("p (a b) -> p a b", a=ap.shape[1]), ap, [], [gb("ft0")])
                else:
                    COPYV(ft[0][:, 0:n], ap, [], [gb("ft0")])
                DMAS(out_d[0, r0:r0 + 128, c0:c0 + n], ft[0][:, 0:n], [gb("ft0")], [gb("outd")])
        S.finish("sp")
        with nc.Block() as block:
            S.replay(block)
    return nc


def _consts():
    ident = np.eye(128, dtype=np.float32)
    pm = np.zeros((128, 128), np.float32)
    for m in range(128):
        i = m % 32
        partner = m + 16 if i < 16 else m - 16
        pm[partner, m] = 1.0
    n = SEQ
    rows = n // 64
    pos_r = np.repeat(np.arange(rows, dtype=np.float32), 64)
    pos_c = np.tile(np.arange(64, dtype=np.float32), rows)
    inv_freq = (10000.0 ** (-np.arange(0, 32, 2, dtype=np.float32) / 32)).astype(np.float32)
    cos_t = np.zeros((128, n), np.float32)
    sin_t = np.zeros((128, n), np.float32)
    for m in range(128):
        axis = (m % 64) // 32
        half = (m % 32) // 16
        i = m % 16
        pos = pos_r if axis == 0 else pos_c
        ang = (pos * inv_freq[i]).astype(np.float32)
        cos_t[m] = np.cos(ang)
        sin_t[m] = np.sin(ang) * (-1.0 if half == 0 else 1.0)
    return ident, pm, np.stack([cos_t, sin_t]).astype(np.float32)


_NC_CACHE = {}


def run_batches(inputs, batch_lists, n_layers=DEPTH):
    NB = len(batch_lists[0])
    key = (NB, n_layers)
    if key not in _NC_CACHE:
        _NC_CACHE[key] = build_nc(NB, n_layers)
    nc = _NC_CACHE[key]
    ident, pm, rope = _consts()
    f = lambda a: np.ascontiguousarray(np.asarray(a, dtype=np.float32))
    shared = {k: f(inputs[k]) for k in (
        "mod_w", "mod_b", "norm1_w", "norm2_w", "w_in", "conv_a_w", "rg_conv_w", "rg_conv_b", "rg_wa", "rg_ba",
        "rg_wx", "rg_bx", "rg_lambda", "da_lambda", "da_subln_w", "w_branch", "w_out", "ffn_w13", "ffn_w2",
        "router_w", "moe_w13", "moe_w2", "final_norm_w")}
    shared["ident"] = ident
    shared["pmat"] = pm
    shared["rope"] = rope
    x = f(inputs["x"])
    ctx = f(inputs["ctx"])
    c = f(inputs["c"])
    c_ctx = f(inputs["c_ctx"])
    in_maps = []
    for bl in batch_lists:
        m = dict(shared)
        m["x"] = np.ascontiguousarray(x[bl])
        m["ctx"] = np.ascontiguousarray(ctx[bl])
        m["cc"] = np.ascontiguousarray(np.concatenate([c[bl], c_ctx[None, :]], axis=0))
        in_maps.append(m)
    res = run_bass_kernel_spmd(nc, in_maps, core_ids=list(range(len(batch_lists))))
    return [r["out"] for r in res.results]


def kernel(**inputs):
    nb = inputs["x"].shape[0] // NCORE
    bl = [list(range(i * nb, (i + 1) * nb)) for i in range(NCORE)]
    outs = run_batches(inputs, bl)
    return np.concatenate(outs, axis=0).astype(np.float32)
```

```python
import contextlib
import math
import numpy as np
import concourse.bass as bass
import concourse.mybir as mybir
from concourse.bass_utils import run_bass_kernel_spmd

F32 = mybir.dt.float32
BF16 = mybir.dt.bfloat16
AF = mybir.ActivationFunctionType
ALU = mybir.AluOpType
AX = mybir.AxisListType

D = 1024
SEQ = 2048
CTX = 256
DEPTH = 2
NCORE = 8
FF = 3584
NEXP = 8
D_IN = 7168
EPS = 1e-6
C_AB, C_AC, C_AX, C_RG, C_RX, C_Q, C_K, C_V, C_GA, C_GR, C_GD = (
    0, 512, 1024, 1536, 2048, 2560, 3072, 3584, 4096, 5120, 6144)


class Buf:
    __slots__ = ("name", "w", "r", "excl")

    def __init__(self, name="", excl=False):
        self.name = name
        self.w = None
        self.r = {}
        self.excl = excl


class Sched:
    ENG = ("pe", "act", "dve", "pool", "sp")
    NSLOT = 12

    def __init__(self, nc):
        self.nc = nc
        self.streams = {e: [] for e in self.ENG}
        self.cnt = {}
        self.seen = {e: {} for e in self.ENG}
        self.sems = {}
        self.slot_rr = {"sp": 0, "pool": 0}

    def alloc_sems(self, stack):
        for e in self.ENG:
            self.sems[e] = stack.enter_context(self.nc.semaphore("s_" + e))
            self.cnt[e] = 0
        for q in ("sp", "pool"):
            for i in range(self.NSLOT):
                k = "%s_d%d" % (q, i)
                self.sems[k] = stack.enter_context(self.nc.semaphore("s_" + k))
                self.cnt[k] = 0

    def _wait(self, eng, key, val):
        if val <= 0 or self.seen[eng].get(key, 0) >= val:
            return
        self.seen[eng][key] = val
        self.streams[eng].append(("wait", key, val))

    def _deps(self, eng, reads, writes):
        for b in reads:
            if b.w is not None and not (eng == "pe" and b.w[0] == "pe"):
                self._wait(eng, b.w[0], b.w[1])
            if b.excl:
                for k, v in b.r.items():
                    if k != eng:
                        self._wait(eng, k, v)
        for b in writes:
            if b.w is not None and not (eng == "pe" and b.w[0] == "pe"):
                self._wait(eng, b.w[0], b.w[1])
            for k, v in b.r.items():
                if not (eng == "pe" and k == "pe"):
                    self._wait(eng, k, v)

    def _mark(self, tok, reads, writes):
        for b in reads:
            b.r[tok[0]] = tok[1]
        for b in writes:
            b.w = tok
            b.r = {}

    def op(self, eng, fn, reads=(), writes=()):
        self._deps(eng, reads, writes)
        self.cnt[eng] += 1
        self.streams[eng].append(("op", fn, eng, 1))
        self._mark((eng, self.cnt[eng]), reads, writes)

    def dma(self, q, fn, reads=(), writes=()):
        self._deps(q, reads, writes)
        s = self.slot_rr[q]
        self.slot_rr[q] = (s + 1) % self.NSLOT
        key = "%s_d%d" % (q, s)
        self._wait(q, key, self.cnt[key])
        self.cnt[key] += 16
        self.streams[q].append(("op", fn, key, 16))
        self._mark((key, self.cnt[key]), reads, writes)

    def barrier(self):
        for e in self.ENG:
            for key, v in self.cnt.items():
                self._wait(e, key, v)

    def finish(self, eng="sp"):
        for key, v in self.cnt.items():
            self._wait(eng, key, v)

    def replay(self, block):
        sems = self.sems

        def run(name, handle):
            for it in self.streams[name]:
                if it[0] == "wait":
                    handle.wait_ge(sems[it[1]], it[2])
                else:
                    it[1](handle).then_inc(sems[it[2]], it[3])

        @block.tensor
        def _(e):
            run("pe", e)

        @block.scalar
        def _(e):
            run("act", e)

        @block.vector
        def _(e):
            run("dve", e)

        @block.gpsimd
        def _(e):
            run("pool", e)

        @block.sync
        def _(e):
            run("sp", e)


def seq(fns):
    def f(e):
        r = None
        for g in fns:
            r = g(e)
        return r
    return f


class _Stop(Exception):
    pass


def build_nc(NB, n_layers=DEPTH, stop=None):
    nc = bass.Bass("TRN2", target_bir_lowering=False)
    ckn = [0]

    def ck(tag=""):
        ckn[0] += 1
        if stop is not None and ckn[0] >= stop:
            print("STOP at checkpoint", ckn[0], tag, flush=True)
            raise _Stop()

    def din(name, shape):
        return nc.dram_tensor(name, list(shape), F32, kind="ExternalInput").ap()

    x_d = din("x", (NB, SEQ, D))
    ctx_d = din("ctx", (NB, CTX, D))
    cc_d = din("cc", (NB + 1, D))
    mod_w = din("mod_w", (DEPTH, D, 6 * D))
    mod_b = din("mod_b", (DEPTH, 6 * D))
    norm1_w = din("norm1_w", (DEPTH, D))
    norm2_w = din("norm2_w", (DEPTH, D))
    w_in = din("w_in", (DEPTH, D, D_IN))
    conv_a_w = din("conv_a_w", (DEPTH, 3, 512))
    rg_conv_w = din("rg_conv_w", (DEPTH, 2, 4, 512))
    rg_conv_b = din("rg_conv_b", (DEPTH, 2, 512))
    rg_wa = din("rg_wa", (DEPTH, 2, 8, 64, 64))
    rg_ba = din("rg_ba", (DEPTH, 2, 8, 64))
    rg_wx = din("rg_wx", (DEPTH, 2, 8, 64, 64))
    rg_bx = din("rg_bx", (DEPTH, 2, 8, 64))
    rg_lambda = din("rg_lambda", (DEPTH, 2, 512))
    da_lambda = din("da_lambda", (DEPTH, 4, 64))
    da_subln_w = din("da_subln_w", (DEPTH, 128))
    w_branch = din("w_branch", (DEPTH, 3, 512, D))
    w_out = din("w_out", (DEPTH, D, D))
    ffn_w13 = din("ffn_w13", (1, D, 2 * FF))
    ffn_w2 = din("ffn_w2", (1, FF, D))
    router_w = din("router_w", (1, D, NEXP))
    moe_w13 = din("moe_w13", (1, NEXP, D, 2 * FF))
    moe_w2 = din("moe_w2", (1, NEXP, FF, D))
    final_norm_w = din("final_norm_w", (D,))
    ident_d = din("ident", (128, 128))
    pmat_d = din("pmat", (128, 128))
    rope_d = din("rope", (2, 128, SEQ))
    out_d = nc.dram_tensor("out", [NB, SEQ, D], F32, kind="ExternalOutput").ap()
    kscr = nc.dram_tensor("kscr", [DEPTH, 4, 128, CTX], BF16, kind="Internal").ap()
    vscr = nc.dram_tensor("vscr", [DEPTH, 4, 128, 2, 128], BF16, kind="Internal").ap()

    S = Sched(nc)
    with contextlib.ExitStack() as st:
        S.alloc_sems(st)

        def sb(name, shape, dt):
            return st.enter_context(nc.sbuf_tensor("sb_" + name, list(shape), dt))

        xT = sb("xT", (128, 8, SEQ), F32)
        uT = sb("uT", (128, 8, SEQ), BF16)
        Y = sb("Y", (128, 24576), BF16)
        ft = [sb("ft%d" % i, (128, 512), F32) for i in range(5)]
        bt = [sb("bt%d" % i, (128, 512), BF16) for i in range(3)]
        PR = sb("PR", (128, 4160), BF16)
        wbuf = [sb("wb%d" % i, (128, 8, 128), BF16) for i in range(2)]
        NWB = 2
        hcar = sb("hcar", (128, 2), F32)
        ident = sb("ident", (128, 128), F32)
        onesb = sb("onesb", (128, 128), BF16)
        pmat = sb("pmat", (128, 128), BF16)
        rgW = sb("rgW", (128, 4, 128), BF16)
        prow = ft[1][:, 0:128]
        pcolA = sb("pcolA", (128, DEPTH, 96), F32)
        pcolB = sb("pcolB", (128, 112), F32)
        ccol = sb("ccol", (128, 8 * (NB + 1)), F32)
        scol = sb("scol", (128, 8, NB + 1), BF16)
        modc = sb("modc", (128, DEPTH, NB + 1, 48), F32)
        wm = sb("wm", (128, 16), F32)
        lamt = ft[2][:, 0:256]
        lamc = sb("lamc", (128, DEPTH, 4), F32)
        cst = sb("cst", (128, 4), F32)
        Kc = PR[:, 0:CTX]
        Vc = PR[:, CTX:2 * CTX].rearrange("p (t d) -> p t d", d=128)
        hsave = sb("hsave", (128, DEPTH, 4, 2), F32)
        rt = sb("rt", (128, 48), F32)
        ps = [st.enter_context(nc.psum_tensor("ps%d" % i, [128, 512], F32)) for i in range(8)]
        PB = [Buf("ps%d" % i, excl=True) for i in range(8)]
        _rem = nc.sbuf_bytes_remaining
        assert 212863 - _rem <= 179500, ("SBUF over the safe limit", 212863 - _rem)

        yA = Y[:, 0:8192].rearrange("p (j t) -> p j t", j=4)
        yR = Y[:, 8192:16384].rearrange("p (j t) -> p j t", j=4)
        yD = Y[:, 16384:24576].rearrange("p (j t) -> p j t", j=4)
        ropec = Y[:, 0:2048]
        ropes = Y[:, 2048:4096]
        qT = Y[:, 4096:6144]
        kT = Y[:, 6144:8192]
        vtok = Y[:, 8192:10240].rearrange("p (c d) -> p c d", d=128)
        qT2 = Y[:, 10240:12288]
        def ffw(i):
            base = i * 12288
            return (Y[:, base:base + 4096].rearrange("p (k n) -> p k n", k=8),
                    Y[:, base + 4096:base + 8192].rearrange("p (k n) -> p k n", k=8),
                    Y[:, base + 8192:base + 12288].rearrange("p (f n) -> p f n", f=4))
        def modst(i):
            return Y[:, i * 4096:(i + 1) * 4096].rearrange("p (k n) -> p k n", k=8)
        iost = Y[:, 16384:24576].bitcast(F32).rearrange("p (t d) -> p t d", t=4)
        WTt = sb("WTt", (128, 16, 8), F32)

        B = {}

        def gb(name):
            if name not in B:
                B[name] = Buf(name)
            return B[name]

        bank_rr = [0]

        def bank(lo=0, hi=8):
            i = lo + bank_rr[0] % (hi - lo)
            bank_rr[0] += 1
            return i

        wb_rr = [0]

        def ACT(out, in_, func, reads, writes, scale=None, bias=None):
            kw = {}
            if scale is not None:
                kw["scale"] = scale
            if bias is not None:
                kw["bias"] = bias
            S.op("act", lambda e: e.activation(out=out, in_=in_, func=func, **kw), reads, writes)

        def TT(out, a, b, op, reads, writes):
            S.op("dve", lambda e: e.tensor_tensor(out=out, in0=a, in1=b, op=op), reads, writes)

        def STT(out, in0, scalar, in1, op0, op1, reads, writes):
            S.op("dve", lambda e: e.scalar_tensor_tensor(out=out, in0=in0, scalar=scalar, in1=in1, op0=op0, op1=op1), reads, writes)

        def TS(out, in0, s1, s2, op0, op1, reads, writes):
            if s2 is None:
                S.op("dve", lambda e: e.tensor_scalar(out=out, in0=in0, scalar1=s1, scalar2=None, op0=op0), reads, writes)
            else:
                S.op("dve", lambda e: e.tensor_scalar(out=out, in0=in0, scalar1=s1, scalar2=s2, op0=op0, op1=op1), reads, writes)

        def RECIP(out, in_, reads, writes):
            S.op("dve", lambda e: e.reciprocal(out=out, in_=in_), reads, writes)

        def COPYV(out, in_, reads, writes):
            S.op("dve", lambda e: e.tensor_copy(out=out, in_=in_), reads, writes)

        def MM(items, reads, writes):
            fns = [(lambda e, o=o, l=l, r=r, a=a, z=z: e.matmul(o, l, r, start=a, stop=z)) for (o, l, r, a, z) in items]
            S.op("pe", seq(fns), reads, writes)

        def TR(out, in_, reads, writes):
            S.op("pe", lambda e: e.transpose(out, in_, ident[:]), list(reads) + [gb("ident")], writes)

        def DMAW(out, in_, reads, writes):
            S.dma("pool", lambda e: e.dma_start(out=out, in_=in_), reads, writes)

        def DMAS(out, in_, reads, writes):
            S.dma("sp", lambda e: e.dma_start(out=out, in_=in_), reads, writes)

        DMAS(ident[:], ident_d[:, :], [], [gb("ident")])
        DMAW(pmat[:], pmat_d[:, :], [], [gb("pmat")])
        S.op("dve", lambda e: e.memset(onesb[:], 1.0), [], [gb("onesb")])
        S.op("dve", lambda e: e.memset(cst[:, 0:1], EPS), [], [gb("cst")])
        S.op("dve", lambda e: e.memset(cst[:, 1:2], 1.0), [], [gb("cst")])
        S.op("dve", lambda e: e.memset(cst[:, 2:3], 0.0), [], [gb("cst")])
        S.op("dve", lambda e: e.memset(rgW[:], 0.0), [], [gb("rgW")])
        S.op("dve", lambda e: e.memset(prow, 0.0), [], [gb("prow")])

        def rows_to_cols(row_specs, nrows, dst):
            for ap, r0, r in row_specs:
                DMAS(prow[r0:r0 + r, :], ap, [], [gb("prow")])
            pb = bank()
            TR(ps[pb][:, 0:128], prow, [gb("prow")], [PB[pb]])
            COPYV(dst, ps[pb][:, 0:nrows], [PB[pb]], [gb("pcol")])

        for l in range(DEPTH):
            specs = [
                (norm1_w[l].rearrange("(r p) -> r p", p=128), 0, 8),
                (norm2_w[l].rearrange("(r p) -> r p", p=128), 8, 8),
                (conv_a_w[l].rearrange("k (j p) -> (k j) p", p=128), 16, 12),
                (rg_conv_w[l].rearrange("d k (j p) -> (d k j) p", p=128), 28, 32),
                (rg_conv_b[l].rearrange("d (j p) -> (d j) p", p=128), 60, 8),
                (rg_ba[l].rearrange("d (j g) i -> (d j) (g i)", g=2), 68, 8),
                (rg_bx[l].rearrange("d (j g) i -> (d j) (g i)", g=2), 76, 8),
                (rg_lambda[l].rearrange("d (j p) -> (d j) p", p=128), 84, 8),
            ]
            rows_to_cols(specs, 92, pcolA[:, l, 0:92])
            ACT(pcolA[:, l, 84:92], pcolA[:, l, 84:92], AF.Exp, [gb("pcol")], [gb("pcol")], scale=-1.0)
            ACT(pcolA[:, l, 84:92], pcolA[:, l, 84:92], AF.Ln, [gb("pcol")], [gb("pcol")], scale=1.0, bias=cst[:, 1:2])
            TS(pcolA[:, l, 84:92], pcolA[:, l, 84:92], -8.0, None, ALU.mult, None, [gb("pcol")], [gb("pcol")])
        specs = [
            (mod_b[0].rearrange("(r p) -> r p", p=128), 0, 48),
            (mod_b[1].rearrange("(r p) -> r p", p=128), 48, 48),
            (final_norm_w.rearrange("(r p) -> r p", p=128), 96, 8),
            (da_subln_w[0].rearrange("(r p) -> r p", p=128), 104, 1),
            (da_subln_w[1].rearrange("(r p) -> r p", p=128), 105, 1),
        ]
        rows_to_cols(specs, 106, pcolB[:, 0:106])
        lam_init = [0.8 - 0.6 * math.exp(-0.3 * l) for l in range(DEPTH)]
        for l in range(DEPTH):
            TS(pcolB[:, 104 + l:105 + l], pcolB[:, 104 + l:105 + l], 1.0 - lam_init[l], None, ALU.mult, None, [gb("pcol")], [gb("pcol")])
        rows_to_cols([(cc_d.rearrange("b (r p) -> (b r) p", p=128), 0, 8 * (NB + 1))], 8 * (NB + 1), ccol[:, :])
        ACT(scol[:].rearrange("p k b -> p b k"), ccol[:].rearrange("p (b k) -> p b k", k=8), AF.Silu, [gb("pcol")], [gb("scol")])
        for l in range(DEPTH):
            DMAS(lamt, da_lambda[l].rearrange("a d -> (a d)").partition_broadcast(128), [], [gb("lamt")])
            TT(lamt[:, 0:64], lamt[:, 0:64], lamt[:, 64:128], ALU.mult, [gb("lamt")], [gb("lamt")])
            TT(lamt[:, 128:192], lamt[:, 128:192], lamt[:, 192:256], ALU.mult, [gb("lamt")], [gb("lamt")])
            S.op("dve", lambda e, l=l: e.reduce_sum(out=lamc[:, l, 1:2], in_=lamt[:, 0:64], axis=AX.X), [gb("lamt")], [gb("lamc")])
            S.op("dve", lambda e, l=l: e.reduce_sum(out=lamc[:, l, 2:3], in_=lamt[:, 128:192], axis=AX.X), [gb("lamt")], [gb("lamc")])
            ACT(lamc[:, l, 1:3], lamc[:, l, 1:3], AF.Exp, [gb("lamc")], [gb("lamc")])
            STT(lamc[:, l, 0:1], lamc[:, l, 2:3], -lam_init[l], lamc[:, l, 1:2], ALU.add, ALU.subtract, [gb("lamc")], [gb("lamc")])

        for l in range(DEPTH):
            pbm = bank()
            for g in range(12):
                stg = modst(g % 2)
                sbuf_b = gb("modst%d" % (g % 2))
                DMAW(stg, mod_w[l][:, g * 512:(g + 1) * 512].rearrange("(k p) n -> p k n", p=128), [], [sbuf_b])
                items = []
                for oc in range(4):
                    occ = g * 4 + oc
                    for kk in range(8):
                        items.append((ps[pbm][:, occ * (NB + 1):(occ + 1) * (NB + 1)],
                                      stg[:, kk, oc * 128:(oc + 1) * 128], scol[:, kk, :], kk == 0, kk == 7))
                MM(items, [sbuf_b, gb("scol")], [PB[pbm]])
            for r in range(NB + 1):
                TT(modc[:, l, r, :], ps[pbm][:, 0:48 * (NB + 1)].rearrange("p (o r) -> p r o", r=NB + 1)[:, r, :],
                   pcolB[:, l * 48:(l + 1) * 48], ALU.add, [PB[pbm], gb("pcol")], [gb("modc")])
        S.barrier()
        try:
            ck("setup")
        except _Stop:
            stop = -1

        def load_w(src_cols_ap):
            i = wb_rr[0] % NWB
            wb_rr[0] += 1
            DMAW(wbuf[i][:], src_cols_ap.rearrange("(k p) n -> p k n", p=128), [], [gb("wb%d" % i)])
            return wbuf[i], gb("wb%d" % i)

        def proj(w, wB, rhs_k, n, reads, lo=0, hi=8):
            pb = bank(lo, hi)
            MM([(ps[pb][:, 0:n], w[:, kk, :], rhs_k(kk), kk == 0, kk == 7) for kk in range(8)],
               [wB] + list(reads), [PB[pb]])
            return pb

        def norm_mod(T, blocks, N, col_w, sh_ap, dst_bf, XB, UB, f32_tap=None):
            for bi in blocks:
                t0 = bi * N
                pb = bank()
                items = []
                for c in range(8):
                    sq = bt[c % 2]
                    sqB = gb("bt%d" % (c % 2))
                    ACT(sq[:, 0:N], xT[:, c, t0:t0 + N], AF.Square, [XB[bi]], [sqB])
                    MM([(ps[pb][:, 0:N], onesb[:], sq[:, 0:N], c == 0, c == 7)], [sqB, gb("onesb")], [PB[pb]])
                rstd = ft[4]
                ACT(rstd[:, 0:N], ps[pb][:, 0:N], AF.Sqrt, [PB[pb], gb("cst")], [gb("ft4")], scale=1.0 / D, bias=cst[:, 0:1])
                RECIP(rstd[:, 0:N], rstd[:, 0:N], [gb("ft4")], [gb("ft4")])
                for c in range(8):
                    tmp = ft[c % 2]
                    tB = gb("ft%d" % (c % 2))
                    TT(tmp[:, 0:N], xT[:, c, t0:t0 + N], rstd[:, 0:N], ALU.mult, [XB[bi], gb("ft4")], [tB])
                    if f32_tap is not None:
                        ACT(tmp[:, 0:N], tmp[:, 0:N], AF.Identity, [tB, gb("wm"), gb("modc")], [tB],
                            scale=col_w[:, c:c + 1], bias=sh_ap[:, c:c + 1])
                        f32_tap(bi, c, tmp, tB)
                        COPYV(dst_bf[:, c, t0:t0 + N], tmp[:, 0:N], [tB], [UB[bi]])
                    else:
                        ACT(dst_bf[:, c, t0:t0 + N], tmp[:, 0:N], AF.Identity, [tB, gb("wm"), gb("modc")], [UB[bi]],
                            scale=col_w[:, c:c + 1], bias=sh_ap[:, c:c + 1])

        def run_pass(b, is_ctx):
            T = CTX if is_ctx else SEQ
            N = min(512, T)
            nblk = T // N
            ntt = N // 128
            r = NB if is_ctx else b
            XB = [gb("X%d" % i) for i in range(nblk)]
            UB = [gb("U%d" % i) for i in range(nblk)]
            YAB = [gb("YA%d" % i) for i in range(nblk)]
            YRB = [gb("YR%d" % i) for i in range(nblk)]
            YDB = [gb("YD%d" % i) for i in range(nblk)]
            src = ctx_d[b] if is_ctx else x_d[b]

            for bi in range(nblk):
                DMAS(iost[:, 0:ntt, :], src[bi * N:(bi + 1) * N, :].rearrange("(t p) d -> p t d", p=128), [], [gb("iost")])
                for c in range(8):
                    pb = bank()
                    for tt in range(ntt):
                        TR(ps[pb][:, tt * 128:(tt + 1) * 128], iost[:, tt, c * 128:(c + 1) * 128], [gb("iost")], [PB[pb]])
                    if c % 2 == 0:
                        ACT(xT[:, c, bi * N:(bi + 1) * N], ps[pb][:, 0:N], AF.Copy, [PB[pb]], [XB[bi]])
                    else:
                        COPYV(xT[:, c, bi * N:(bi + 1) * N], ps[pb][:, 0:N], [PB[pb]], [XB[bi]])

            ck("load")
            for l in range(n_layers):
                partial = is_ctx and (l == DEPTH - 1)
                pc = pcolA[:, l, :]
                mc = modc[:, l, r, :]
                STT(wm[:, 0:8], mc[:, 8:16], 1.0, pc[:, 0:8], ALU.add, ALU.mult, [gb("modc"), gb("pcol")], [gb("wm")])
                STT(wm[:, 8:16], mc[:, 32:40], 1.0, pc[:, 8:16], ALU.add, ALU.mult, [gb("modc"), gb("pcol")], [gb("wm")])
                norm_mod(T, range(nblk), N, wm[:, 0:8], mc[:, 0:8], uT, XB, UB)

                ck("norm1")

                def u_k(bi):
                    return lambda kk: uT[:, kk, bi * N:(bi + 1) * N]


                ck("rgW")
                if not is_ctx:
                    DMAW(ropec, rope_d[0], [], [gb("rope")])
                    DMAW(ropes, rope_d[1], [], [gb("rope")])
                S.op("dve", lambda e, T=T: e.memset(qT[64:128, 0:T], 0.0), [], [gb("qT")])
                S.op("dve", lambda e, T=T: e.memset(qT2[0:64, 0:T], 0.0), [], [gb("qT")])
                for h in range(4):
                    for which, c0, dstT, dB in ((0, C_K, kT, gb("kT")), (1, C_Q, qT, gb("qT"))):
                        if partial and which == 1:
                            continue
                        w, wB = load_w(w_in[l][:, c0 + h * 128:c0 + (h + 1) * 128])
                        if h == 0 and which == 0:
                            ck("kload")
                        for bi in range(nblk):
                            pb = proj(w, wB, u_k(bi), N, [UB[bi]], 0, 4)
                            sl = slice(bi * N, (bi + 1) * N)
                            if h == 0 and which == 0 and bi == 0:
                                ck("kproj")
                            if is_ctx:
                                if which == 0:
                                    ACT(dstT[:, sl], ps[pb][:, 0:N], AF.Copy, [PB[pb]], [dB])
                                    if h == 0:
                                        ck("kact")
                                    DMAS(kscr[l, h], dstT[:, sl], [dB], [gb("kscr")])
                                    if h == 0:
                                        ck("ksave")
                                else:
                                    ACT(qT[0:64, sl], ps[pb][0:64, 0:N], AF.Copy, [PB[pb]], [dB])
                                    ACT(qT2[64:128, sl], ps[pb][64:128, 0:N], AF.Copy, [PB[pb]], [dB])
                            else:
                                raw = bt[2]
                                rB = gb("bt2")
                                ACT(raw[:, 0:N], ps[pb][:, 0:N], AF.Copy, [PB[pb]], [rB])
                                pb2 = bank(0, 4)
                                MM([(ps[pb2][:, 0:N], pmat[:], raw[:, 0:N], True, True)], [rB, gb("pmat")], [PB[pb2]])
                                if h == 0 and which == 0:
                                    ck("ropemm%d" % bi)
                                t1 = ft[2]
                                t2 = ft[3]
                                ACT(t1[:, 0:N], ps[pb][:, 0:N], AF.Copy, [PB[pb]], [gb("ft2")])
                                TT(t1[:, 0:N], t1[:, 0:N], ropec[:, sl], ALU.mult, [gb("ft2"), gb("rope")], [gb("ft2")])
                                if h == 0 and which == 0:
                                    ck("ropet1%d" % bi)
                                ACT(t2[:, 0:N], ps[pb2][:, 0:N], AF.Copy, [PB[pb2]], [gb("ft3")])
                                TT(t2[:, 0:N], t2[:, 0:N], ropes[:, sl], ALU.mult, [gb("ft3"), gb("rope")], [gb("ft3")])
                                if which == 0:
                                    TT(dstT[:, sl], t1[:, 0:N], t2[:, 0:N], ALU.add, [gb("ft2"), gb("ft3")], [dB])
                                else:
                                    TT(qT[0:64, sl], t1[0:64, 0:N], t2[0:64, 0:N], ALU.add, [gb("ft2"), gb("ft3")], [dB])
                                    TT(qT2[64:128, sl], t1[64:128, 0:N], t2[64:128, 0:N], ALU.add, [gb("ft2"), gb("ft3")], [dB])
                            if h == 0 and which == 0 and not is_ctx:
                                ck("kblk%d" % bi)
                        if h == 0 and which == 0 and not is_ctx:
                            ck("kdone")
                    if h == 0:
                        ck("kq")
                    w, wB = load_w(w_in[l][:, C_V + h * 128:C_V + (h + 1) * 128])
                    for tg in range(T // 512 if T >= 512 else 1):
                        pb = bank(0, 4)
                        nt = min(4, T // 128)
                        items = []
                        for tt in range(nt):
                            tok0 = (tg * 4 + tt) * 128
                            for kk in range(8):
                                items.append((ps[pb][:, tt * 128:(tt + 1) * 128], uT[:, kk, tok0:tok0 + 128], w[:, kk, :], kk == 0, kk == 7))
                        MM(items, [wB] + UB, [PB[pb]])
                        ACT(vtok[:, tg * 4:tg * 4 + nt, :], ps[pb][:, 0:nt * 128].rearrange("p (t d) -> p t d", d=128), AF.Copy, [PB[pb]], [gb("vtok")])
                        if is_ctx:
                            DMAS(vscr[l, h], vtok[:, 0:2, :], [gb("vtok")], [gb("vscr")])
                    if h == 0:
                        ck("v")
                    if partial:
                        continue
                    keys = []
                    if not is_ctx:
                        DMAS(Kc, kscr[l, h], [gb("kscr")], [gb("Kc")])
                        DMAS(Vc, vscr[l, h], [gb("vscr")], [gb("Vc")])
                        for kc in range(2):
                            keys.append((Kc[:, kc * 128:(kc + 1) * 128], Vc[:, kc, :], [gb("Kc"), gb("Vc")]))
                    for kc in range(T // 128):
                        keys.append((kT[:, kc * 128:(kc + 1) * 128], vtok[:, kc, :], [gb("kT"), gb("vtok")]))
                    nk = len(keys)
                    for bi in range(nblk):
                        sl = slice(bi * N, (bi + 1) * N)
                        for ki, (kap, vap, kB) in enumerate(keys):
                            for m in range(2):
                                pbs = m * 2 + (ki % 2)
                                qsrc = qT if m == 0 else qT2
                                MM([(ps[pbs][:, 0:N], kap, qsrc[:, sl], True, True)], kB + [gb("qT")], [PB[pbs]])
                                P = bt[m]
                                PBf = gb("bt%d" % m)
                                ACT(P[:, 0:N], ps[pbs][:, 0:N], AF.Exp, [PB[pbs]], [PBf], scale=0.125)
                                MM([(ps[4 + 2 * m][:, 0:N], vap, P[:, 0:N], ki == 0, ki == nk - 1),
                                    (ps[5 + 2 * m][:, 0:N], onesb[:], P[:, 0:N], ki == 0, ki == nk - 1)],
                                   kB + [PBf, gb("onesb")], [PB[4 + 2 * m], PB[5 + 2 * m]])
                        if h == 0 and bi == 0:
                            ck("scores")
                        RECIP(ft[0][:, 0:N], ps[5][:, 0:N], [PB[5]], [gb("ft0")])
                        TT(ft[0][:, 0:N], ps[4][:, 0:N], ft[0][:, 0:N], ALU.mult, [PB[4], gb("ft0")], [gb("ft0")])
                        RECIP(ft[1][:, 0:N], ps[7][:, 0:N], [PB[7]], [gb("ft1")])
                        TT(ft[1][:, 0:N], ps[6][:, 0:N], ft[1][:, 0:N], ALU.mult, [PB[6], gb("ft1")], [gb("ft1")])
                        STT(ft[0][:, 0:N], ft[1][:, 0:N], lamc[:, l, 0:1], ft[0][:, 0:N], ALU.mult, ALU.add, [gb("ft1"), gb("ft0"), gb("lamc")], [gb("ft0")])
                        ACT(bt[2][:, 0:N], ft[0][:, 0:N], AF.Square, [gb("ft0")], [gb("bt2")])
                        MM([(ps[0][:, 0:N], onesb[:], bt[2][:, 0:N], True, True)], [gb("bt2"), gb("onesb")], [PB[0]])
                        ACT(ft[1][:, 0:N], ps[0][:, 0:N], AF.Sqrt, [PB[0], gb("cst")], [gb("ft1")], scale=1.0 / 128, bias=cst[:, 0:1])
                        RECIP(ft[1][:, 0:N], ft[1][:, 0:N], [gb("ft1")], [gb("ft1")])
                        TT(ft[0][:, 0:N], ft[0][:, 0:N], ft[1][:, 0:N], ALU.mult, [gb("ft0"), gb("ft1")], [gb("ft0")])
                        ACT(yD[:, h, sl], ft[0][:, 0:N], AF.Identity, [gb("ft0"), gb("pcol")], [YDB[bi]], scale=pcolB[:, 104 + l:105 + l])
                S.barrier()
                ck("attn")

                if not partial:
                    cx = PR[:, 0:T + 2]
                    S.op("dve", lambda e: e.memset(PR[:, 0:1], 0.0), [], [gb("cx")])
                    S.op("dve", lambda e, T=T: e.memset(PR[:, T + 1:T + 2], 0.0), [], [gb("cx")])
                    for j in range(4):
                        acst = PR[:, 2112:2112 + T]
                        wc, wcB = load_w(w_in[l][:, C_AC + j * 128:C_AC + (j + 1) * 128])
                        for bi in range(nblk):
                            pbc = proj(wc, wcB, u_k(bi), N, [UB[bi]])
                            ACT(acst[:, bi * N:(bi + 1) * N], ps[pbc][:, 0:N], AF.Copy, [PB[pbc]], [gb("acst")])
                        wx_, wxB = load_w(w_in[l][:, C_AX + j * 128:C_AX + (j + 1) * 128])
                        for bi in range(nblk):
                            pbx = proj(wx_, wxB, u_k(bi), N, [UB[bi]])
                            TT(PR[:, 1 + bi * N:1 + (bi + 1) * N], ps[pbx][:, 0:N], acst[:, bi * N:(bi + 1) * N], ALU.mult, [PB[pbx], gb("acst")], [gb("cx")])
                        wb_, wbB = load_w(w_in[l][:, C_AB + j * 128:C_AB + (j + 1) * 128])
                        for bi in range(nblk):
                            pbb = proj(wb_, wbB, u_k(bi), N, [UB[bi]])
                            t = ft[1]
                            o = bi * N
                            TS(t[:, 0:N], PR[:, o:o + N], pc[:, 16 + j:17 + j], None, ALU.mult, None, [gb("cx"), gb("pcol")], [gb("ft1")])
                            STT(t[:, 0:N], PR[:, o + 1:o + 1 + N], pc[:, 20 + j:21 + j], t[:, 0:N], ALU.mult, ALU.add, [gb("cx"), gb("pcol"), gb("ft1")], [gb("ft1")])
                            STT(t[:, 0:N], PR[:, o + 2:o + 2 + N], pc[:, 24 + j:25 + j], t[:, 0:N], ALU.mult, ALU.add, [gb("cx"), gb("pcol"), gb("ft1")], [gb("ft1")])
                            TT(yA[:, j, o:o + N], ps[pbb][:, 0:N], t[:, 0:N], ALU.mult, [PB[pbb], gb("ft1")], [YAB[bi]])
                    S.barrier()
                    ck("mixA")

                xr = PR[:, 0:T + 6]
                hF = PR[:, 2112:2112 + T]
                for j in range(4):
                    S.op("dve", lambda e: e.memset(PR[:, 0:3], 0.0), [], [gb("xr")])
                    S.op("dve", lambda e, T=T: e.memset(PR[:, T + 3:T + 6], 0.0), [], [gb("xr")])
                    for which, wsrc in enumerate((rg_wa, rg_wx)):
                        for d in range(2):
                            for g in range(2):
                                DMAW(rgW[g * 64:(g + 1) * 64, which * 2 + d, g * 64:(g + 1) * 64], wsrc[l, d, 2 * j + g], [], [gb("rgW")])
                    w, wB = load_w(w_in[l][:, C_RX + j * 128:C_RX + (j + 1) * 128])
                    for bi in range(nblk):
                        pb = proj(w, wB, u_k(bi), N, [UB[bi]])
                        ACT(PR[:, 3 + bi * N:3 + (bi + 1) * N], ps[pb][:, 0:N], AF.Copy, [PB[pb]], [gb("xr")])
                    if not partial:
                        wg, wgB = load_w(w_in[l][:, C_RG + j * 128:C_RG + (j + 1) * 128])
                    for d in range(2):
                        order = list(range(nblk)) if d == 0 else list(range(nblk - 1, -1, -1))
                        prev_h = None
                        for oi, bi in enumerate(order):
                            o = bi * N + (0 if d == 0 else 3)
                            xc = bt[2]
                            cw = 28 + d * 16
                            TS(ft[0][:, 0:N], PR[:, o:o + N], pc[:, cw + j:cw + j + 1], pc[:, 60 + d * 4 + j:61 + d * 4 + j], ALU.mult, ALU.add, [gb("xr"), gb("pcol")], [gb("ft0")])
                            for k in range(1, 4):
                                dst = xc[:, 0:N] if k == 3 else ft[0][:, 0:N]
                                dB = gb("bt2") if k == 3 else gb("ft0")
                                STT(dst, PR[:, o + k:o + k + N], pc[:, cw + 4 * k + j:cw + 4 * k + j + 1], ft[0][:, 0:N], ALU.mult, ALU.add, [gb("xr"), gb("pcol"), gb("ft0")], [dB])
                            pa = bank()
                            pi = bank()
                            MM([(ps[pa][:, 0:N], rgW[:, d, :], xc[:, 0:N], True, True)], [gb("rgW"), gb("bt2")], [PB[pa]])
                            MM([(ps[pi][:, 0:N], rgW[:, 2 + d, :], xc[:, 0:N], True, True)], [gb("rgW"), gb("bt2")], [PB[pi]])
                            ACT(ft[1][:, 0:N], ps[pa][:, 0:N], AF.Sigmoid, [PB[pa], gb("pcol")], [gb("ft1")], scale=1.0, bias=pc[:, 68 + d * 4 + j:69 + d * 4 + j])
                            ACT(ft[2][:, 0:N], ps[pi][:, 0:N], AF.Sigmoid, [PB[pi], gb("pcol")], [gb("ft2")], scale=1.0, bias=pc[:, 76 + d * 4 + j:77 + d * 4 + j])
                            ACT(ft[1][:, 0:N], ft[1][:, 0:N], AF.Exp, [gb("ft1"), gb("pcol")], [gb("ft1")], scale=pc[:, 84 + d * 4 + j:85 + d * 4 + j])
                            ACT(ft[3][:, 0:N], ft[1][:, 0:N], AF.Square, [gb("ft1")], [gb("ft3")])
                            ACT(ft[3][:, 0:N], ft[3][:, 0:N], AF.Sqrt, [gb("ft3"), gb("cst")], [gb("ft3")], scale=-1.0, bias=cst[:, 1:2])
                            TT(ft[2][:, 0:N], ft[2][:, 0:N], xc[:, 0:N], ALU.mult, [gb("ft2"), gb("bt2")], [gb("ft2")])
                            TT(ft[2][:, 0:N], ft[2][:, 0:N], ft[3][:, 0:N], ALU.mult, [gb("ft2"), gb("ft3")], [gb("ft2")])
                            hcur = ft[4]
                            hB = gb("ft4")
                            if prev_h is None:
                                init = cst[:, 2:3] if is_ctx else hsave[:, l, j, d:d + 1]
                                iB = [gb("cst"), gb("hsave")]
                            else:
                                COPYV(hcar[:, d:d + 1], hcur[:, N - 1:N] if d == 0 else hcur[:, 0:1], [hB], [gb("hcar")])
                                init = hcar[:, d:d + 1]
                                iB = [gb("hcar")]
                            if d == 0:
                                S.op("dve", lambda e, hcur=hcur, init=init, N=N: e.tensor_tensor_scan(out=hcur[:, 0:N], data0=ft[1][:, 0:N], data1=ft[2][:, 0:N], initial=init, op0=ALU.mult, op1=ALU.add),
                                     [gb("ft1"), gb("ft2")] + iB, [hB])
                                if not partial:
                                    ACT(hF[:, bi * N:(bi + 1) * N], hcur[:, 0:N], AF.Copy, [hB], [gb("hF")])
                            else:
                                S.op("dve", lambda e, hcur=hcur, init=init, N=N: e.tensor_tensor_scan(out=hcur[:, N - 1::-1] if False else hcur[:, 0:N][:, ::-1], data0=ft[1][:, 0:N][:, ::-1], data1=ft[2][:, 0:N][:, ::-1], initial=init, op0=ALU.mult, op1=ALU.add),
                                     [gb("ft1"), gb("ft2")] + iB, [hB])
                                if not partial:
                                    pg = proj(wg, wgB, u_k(bi), N, [UB[bi]])
                                    ACT(ft[3][:, 0:N], ps[pg][:, 0:N], AF.Gelu_apprx_tanh, [PB[pg]], [gb("ft3")])
                                    TT(ft[0][:, 0:N], hcur[:, 0:N], hF[:, bi * N:(bi + 1) * N], ALU.add, [hB, gb("hF")], [gb("ft0")])
                                    TT(yR[:, j, bi * N:(bi + 1) * N], ft[0][:, 0:N], ft[3][:, 0:N], ALU.mult, [gb("ft0"), gb("ft3")], [YRB[bi]])
                            prev_h = (hcur, hB)
                        if is_ctx:
                            last = prev_h[0][:, N - 1:N] if d == 0 else prev_h[0][:, 0:1]
                            COPYV(hsave[:, l, j, d:d + 1], last, [prev_h[1]], [gb("hsave")])
                S.barrier()
                ck("mixB")
                if partial:
                    continue

                mT = PR[:, 0:4096].rearrange("p (c n) -> p c n", c=8)
                for bi in range(nblk):
                    sl = slice(bi * N, (bi + 1) * N)
                    for c in range(8):
                        for br, (c0, ysrc, yB) in enumerate(((C_GA, yA, YAB), (C_GR, yR, YRB), (C_GD, yD, YDB))):
                            w, wB = load_w(w_in[l][:, c0 + c * 128:c0 + (c + 1) * 128])
                            pg = proj(w, wB, u_k(bi), N, [UB[bi]])
                            i = wb_rr[0] % NWB
                            wb_rr[0] += 1
                            DMAW(wbuf[i][:, 0:4, :], w_branch[l, br][:, c * 128:(c + 1) * 128].rearrange("(k p) n -> p k n", p=128), [], [gb("wb%d" % i)])
                            pbr = bank()
                            MM([(ps[pbr][:, 0:N], wbuf[i][:, kk, :], ysrc[:, kk, sl], kk == 0, kk == 3) for kk in range(4)],
                               [gb("wb%d" % i), yB[bi]], [PB[pbr]])
                            sg = ft[br % 2]
                            sgB = gb("ft%d" % (br % 2))
                            ACT(sg[:, 0:N], ps[pg][:, 0:N], AF.Sigmoid, [PB[pg]], [sgB])
                            if br == 0:
                                TT(ft[2][:, 0:N], ps[pbr][:, 0:N], sg[:, 0:N], ALU.mult, [PB[pbr], sgB], [gb("ft2")])
                            else:
                                TT(ft[3][:, 0:N], ps[pbr][:, 0:N], sg[:, 0:N], ALU.mult, [PB[pbr], sgB], [gb("ft3")])
                                dst = mT[:, c, 0:N] if br == 2 else ft[2][:, 0:N]
                                dB = gb("mT") if br == 2 else gb("ft2")
                                TT(dst, ft[2][:, 0:N], ft[3][:, 0:N], ALU.add, [gb("ft2"), gb("ft3")], [dB])
                    for c2 in range(8):
                        w, wB = load_w(w_out[l][:, c2 * 128:(c2 + 1) * 128])
                        py = proj(w, wB, lambda kk: mT[:, kk, 0:N], N, [gb("mT")])
                        STT(xT[:, c2, sl], ps[py][:, 0:N], mc[:, 16 + c2:17 + c2], xT[:, c2, sl], ALU.mult, ALU.add, [PB[py], gb("modc"), XB[bi]], [XB[bi]])
                S.barrier()
                ck("merge")

                is_moe = (l % 2 == 1)
                hT = uT
                if is_moe:
                    rw = ft[3]
                    DMAS(rw[:, 0:64].rearrange("p (k e) -> p k e", e=8), router_w[0].rearrange("(k p) e -> p k e", p=128), [], [gb("ft3")])
                    WT = WTt[:]
                    pr_bank = [None]

                    def tap(bi, c, tmp, tB):
                        if c == 0:
                            pr_bank[0] = [bank() for _ in range(ntt)]
                        pbs_ = pr_bank[0]
                        MM([(ps[pbs_[tt]][:, 0:8], tmp[:, tt * 128:(tt + 1) * 128], rw[:, c * 8:(c + 1) * 8], c == 0, c == 7) for tt in range(ntt)],
                           [tB, gb("ft3")], [PB[p_] for p_ in pbs_])
                        if c == 7:
                            for tt in range(ntt):
                                gt = bi * ntt + tt
                                L = rt[:, 0:8]
                                pb = pbs_[tt]
                                COPYV(L, ps[pb][:, 0:8], [PB[pb]], [gb("rt")])
                                S.op("dve", lambda e: e.reduce_max(out=rt[:, 40:41], in_=rt[:, 0:8], axis=AX.X), [gb("rt")], [gb("rt")])
                                TS(rt[:, 8:16], rt[:, 0:8], rt[:, 40:41], None, ALU.is_equal, None, [gb("rt")], [gb("rt")])
                                STT(rt[:, 16:24], rt[:, 8:16], -1e30, rt[:, 0:8], ALU.mult, ALU.add, [gb("rt")], [gb("rt")])
                                S.op("dve", lambda e: e.reduce_max(out=rt[:, 41:42], in_=rt[:, 16:24], axis=AX.X), [gb("rt")], [gb("rt")])
                                TS(rt[:, 24:32], rt[:, 16:24], rt[:, 41:42], None, ALU.is_equal, None, [gb("rt")], [gb("rt")])
                                TT(rt[:, 42:43], rt[:, 41:42], rt[:, 40:41], ALU.subtract, [gb("rt")], [gb("rt")])
                                ACT(rt[:, 42:43], rt[:, 42:43], AF.Exp, [gb("rt")], [gb("rt")])
                                TS(rt[:, 43:44], rt[:, 42:43], 1.0, None, ALU.add, None, [gb("rt")], [gb("rt")])
                                RECIP(rt[:, 43:44], rt[:, 43:44], [gb("rt")], [gb("rt")])
                                TT(rt[:, 44:45], rt[:, 42:43], rt[:, 43:44], ALU.mult, [gb("rt")], [gb("rt")])
                                TS(rt[:, 32:40], rt[:, 8:16], rt[:, 43:44], None, ALU.mult, None, [gb("rt")], [gb("rt")])
                                STT(WT[:, gt, :], rt[:, 24:32], rt[:, 44:45], rt[:, 32:40], ALU.mult, ALU.add, [gb("rt")], [gb("WT")])
                    norm_mod(T, range(nblk), N, wm[:, 8:16], mc[:, 24:32], hT, XB, UB, f32_tap=tap)
                else:
                    norm_mod(T, range(nblk), N, wm[:, 8:16], mc[:, 24:32], hT, XB, UB)

                act = [PR[:, i * 2048:(i + 1) * 2048].rearrange("p (f n) -> p f n", f=4) for i in range(2)]
                experts = range(NEXP) if is_moe else [None]
                grp = 0
                for ex in experts:
                    w13 = moe_w13[0, ex] if is_moe else ffn_w13[0]
                    w2 = moe_w2[0, ex] if is_moe else ffn_w2[0]
                    for gi in range(7):
                        Wg, Wu, W2 = ffw(grp % 2)
                        fB = gb("ffw%d" % (grp % 2))
                        grp += 1
                        DMAW(Wg, w13[:, gi * 512:(gi + 1) * 512].rearrange("(k p) n -> p k n", p=128), [], [fB])
                        DMAW(Wu, w13[:, FF + gi * 512:FF + (gi + 1) * 512].rearrange("(k p) n -> p k n", p=128), [], [fB])
                        DMAW(W2, w2[gi * 512:(gi + 1) * 512, :].rearrange("(f p) n -> p f n", p=128), [], [fB])
                        for bi in range(nblk):
                            sl = slice(bi * N, (bi + 1) * N)
                            a_t = act[bi % 2]
                            aB = gb("act%d" % (bi % 2))
                            if is_moe:
                                pgb = bank()
                                for tt in range(ntt):
                                    gt = bi * ntt + tt
                                    TS(ft[4][:, tt * 128:(tt + 1) * 128], onesb[:], WT[:, gt, ex:ex + 1], None, ALU.mult, None, [gb("onesb"), gb("WT")], [gb("ft4")])
                                MM([(ps[pgb][:, tt * 128:(tt + 1) * 128], ft[4][:, tt * 128:(tt + 1) * 128], ident[:], True, True) for tt in range(ntt)],
                                   [gb("ft4"), gb("ident")], [PB[pgb]])
                                ACT(ft[3][:, 0:N], ps[pgb][:, 0:N], AF.Copy, [PB[pgb]], [gb("ft3")])
                            for f in range(4):
                                pg = bank()
                                MM([(ps[pg][:, 0:N], Wg[:, kk, f * 128:(f + 1) * 128], hT[:, kk, sl], kk == 0, kk == 7) for kk in range(8)], [fB, UB[bi]], [PB[pg]])
                                pu = bank()
                                MM([(ps[pu][:, 0:N], Wu[:, kk, f * 128:(f + 1) * 128], hT[:, kk, sl], kk == 0, kk == 7) for kk in range(8)], [fB, UB[bi]], [PB[pu]])
                                sg = bt[f % 2]
                                sgB = gb("bt%d" % (f % 2))
                                ACT(sg[:, 0:N], ps[pg][:, 0:N], AF.Silu, [PB[pg]], [sgB])
                                if is_moe:
                                    tmpf = ft[f % 2]
                                    tfB = gb("ft%d" % (f % 2))
                                    TT(tmpf[:, 0:N], ps[pu][:, 0:N], sg[:, 0:N], ALU.mult, [PB[pu], sgB], [tfB])
                                    TT(a_t[:, f, 0:N], tmpf[:, 0:N], ft[3][:, 0:N], ALU.mult, [tfB, gb("ft3")], [aB])
                                else:
                                    TT(a_t[:, f, 0:N], ps[pu][:, 0:N], sg[:, 0:N], ALU.mult, [PB[pu], sgB], [aB])
                            for c2 in range(8):
                                py = bank()
                                MM([(ps[py][:, 0:N], W2[:, f, c2 * 128:(c2 + 1) * 128], a_t[:, f, 0:N], f == 0, f == 3) for f in range(4)], [fB, aB], [PB[py]])
                                STT(xT[:, c2, sl], ps[py][:, 0:N], mc[:, 40 + c2:41 + c2], xT[:, c2, sl], ALU.mult, ALU.add, [PB[py], gb("modc"), XB[bi]], [XB[bi]])
                S.barrier()
                ck("ffn")

            if is_ctx:
                return
            for bi in range(nblk):
                t0 = bi * N
                pb = bank()
                for c in range(8):
                    sq = bt[c % 2]
                    sqB = gb("bt%d" % (c % 2))
                    ACT(sq[:, 0:N], xT[:, c, t0:t0 + N], AF.Square, [XB[bi]], [sqB])
                    MM([(ps[pb][:, 0:N], onesb[:], sq[:, 0:N], c == 0, c == 7)], [sqB, gb("onesb")], [PB[pb]])
                rstd = ft[4]
                ACT(rstd[:, 0:N], ps[pb][:, 0:N], AF.Sqrt, [PB[pb], gb("cst")], [gb("ft4")], scale=1.0 / D, bias=cst[:, 0:1])
                RECIP(rstd[:, 0:N], rstd[:, 0:N], [gb("ft4")], [gb("ft4")])
                for c in range(8):
                    tmp = ft[c % 2]
                    tB = gb("ft%d" % (c % 2))
                    STT(tmp[:, 0:N], xT[:, c, t0:t0 + N], pcolB[:, 96 + c:97 + c], rstd[:, 0:N], ALU.mult, ALU.mult, [XB[bi], gb("ft4"), gb("pcol")], [tB])
                    pt = bank()
                    for tt in range(ntt):
                        TR(ps[pt][:, tt * 128:(tt + 1) * 128], tmp[:, tt * 128:(tt + 1) * 128], [tB], [PB[pt]])
                    if c % 2 == 0:
                        ACT(iost[:, 0:ntt, c * 128:(c + 1) * 128], ps[pt][:, 0:N].rearrange("p (t d) -> p t d", d=128), AF.Copy, [PB[pt]], [gb("iost")])
                    else:
                        COPYV(iost[:, 0:ntt, c * 128:(c + 1) * 128], ps[pt][:, 0:N].rearrange("p (t d) -> p t d", d=128), [PB[pt]], [gb("iost")])
                DMAS(out_d[b, t0:t0 + N, :].rearrange("(t p) d -> p t d", p=128), iost[:, 0:ntt, :], [gb("iost")], [gb("outd")])
            S.barrier()

        try:
            if stop != -1:
                for b in range(NB):
                    run_pass(b, True)
                    run_pass(b, False)
        except _Stop:
            S.barrier()
            dbg = []
            for c in range(8):
                dbg.append((uT[:, c, 0:256], c, 256))
                dbg.append((xT[:, c, 0:256], 20 + c, 256))
                dbg.append((xT[:, c, 1792:2048], 40 + c, 256))
            for j in range(4):
                dbg.append((yD[:, j, 0:256], 8 + j, 256))
                dbg.append((yA[:, j, 0:256], 12 + j, 256))
                dbg.append((yR[:, j, 0:256], 16 + j, 256))
                dbg.append((yD[:, j, 1792:2048], 32 + j, 256))
                dbg.append((yR[:, j, 1792:2048], 36 + j, 256))
                dbg.append((yA[:, j, 1792:2048], 53 + j, 256))
            dbg += [(kT[:, 0:256], 28, 256), (qT[:, 0:256], 29, 256), (qT2[:, 0:256], 30, 256),
                    (vtok[:, 0:2, :], 31, 256),
                    (modc[:].rearrange("p l r o -> p (l r o)"), 48, DEPTH * (NB + 1) * 48), (wm[:], 49, 16),
                    (pcolA[:].rearrange("p l o -> p (l o)"), 50, 192), (pcolB[:], 51, 112),
                    (hsave[:].rearrange("p l j d -> p (l j d)"), 52, 16), (WTt[:].rearrange("p t e -> p (t e)"), 57, 128),
                    (lamc[:].rearrange("p l o -> p (l o)"), 58, 8)]
            for ap, k, n in dbg:
                r0 = (k // 4) * 128
                c0 = (k % 4) * 256
                if len(ap.shape) == 3:
                    COPYV(ft[0][:, 0:n].rearrange("p (a b) -> p a b", a=ap.shape[1]), ap, [], [gb("ft0")])
                else:
                    COPYV(ft[0][:, 0:n], ap, [], [gb("ft0")])
                DMAS(out_d[0, r0:r0 + 128, c0:c0 + n], ft[0][:, 0:n], [gb("ft0")], [gb("outd")])
        S.finish("sp")
        with nc.Block() as block:
            S.replay(block)
    return nc


def _consts():
    ident = np.eye(128, dtype=np.float32)
    pm = np.zeros((128, 128), np.float32)
    for m in range(128):
        i = m % 32
        partner = m + 16 if i < 16 else m - 16
        pm[partner, m] = 1.0
    n = SEQ
    rows = n // 64
    pos_r = np.repeat(np.arange(rows, dtype=np.float32), 64)
    pos_c = np.tile(np.arange(64, dtype=np.float32), rows)
    inv_freq = (10000.0 ** (-np.arange(0, 32, 2, dtype=np.float32) / 32)).astype(np.float32)
    cos_t = np.zeros((128, n), np.float32)
    sin_t = np.zeros((128, n), np.float32)
    for m in range(128):
        axis = (m % 64) // 32
        half = (m % 32) // 16
        i = m % 16
        pos = pos_r if axis == 0 else pos_c
        ang = (pos * inv_freq[i]).astype(np.float32)
        cos_t[m] = np.cos(ang)
        sin_t[m] = np.sin(ang) * (-1.0 if half == 0 else 1.0)
    return ident, pm, np.stack([cos_t, sin_t]).astype(np.float32)


_NC_CACHE = {}


def run_batches(inputs, batch_lists, n_layers=DEPTH):
    NB = len(batch_lists[0])
    key = (NB, n_layers)
    if key not in _NC_CACHE:
        _NC_CACHE[key] = build_nc(NB, n_layers)
    nc = _NC_CACHE[key]
    ident, pm, rope = _consts()
    f = lambda a: np.ascontiguousarray(np.asarray(a, dtype=np.float32))
    shared = {k: f(inputs[k]) for k in (
        "mod_w", "mod_b", "norm1_w", "norm2_w", "w_in", "conv_a_w", "rg_conv_w", "rg_conv_b", "rg_wa", "rg_ba",
        "rg_wx", "rg_bx", "rg_lambda", "da_lambda", "da_subln_w", "w_branch", "w_out", "ffn_w13", "ffn_w2",
        "router_w", "moe_w13", "moe_w2", "final_norm_w")}
    shared["ident"] = ident
    shared["pmat"] = pm
    shared["rope"] = rope
    x = f(inputs["x"])
    ctx = f(inputs["ctx"])
    c = f(inputs["c"])
    c_ctx = f(inputs["c_ctx"])
    in_maps = []
    for bl in batch_lists:
        m = dict(shared)
        m["x"] = np.ascontiguousarray(x[bl])
        m["ctx"] = np.ascontiguousarray(ctx[bl])
        m["cc"] = np.ascontiguousarray(np.concatenate([c[bl], c_ctx[None, :]], axis=0))
        in_maps.append(m)
    res = run_bass_kernel_spmd(nc, in_maps, core_ids=list(range(len(batch_lists))))
    return [r["out"] for r in res.results]


def kernel(**inputs):
    nb = inputs["x"].shape[0] // NCORE
    bl = [list(range(i * nb, (i + 1) * nb)) for i in range(NCORE)]
    outs = run_batches(inputs, bl)
    return np.concatenate(outs, axis=0).astype(np.float32)
```

```python
import contextlib
import math
import numpy as np
import concourse.bass as bass
import concourse.mybir as mybir
from concourse.bass_utils import run_bass_kernel_spmd

F32 = mybir.dt.float32
BF16 = mybir.dt.bfloat16
AF = mybir.ActivationFunctionType
ALU = mybir.AluOpType
AX = mybir.AxisListType

D = 1024
SEQ = 2048
CTX = 256
DEPTH = 2
NCORE = 8
FF = 3584
NEXP = 8
D_IN = 7168
EPS = 1e-6
C_AB, C_AC, C_AX, C_RG, C_RX, C_Q, C_K, C_V, C_GA, C_GR, C_GD = (
    0, 512, 1024, 1536, 2048, 2560, 3072, 3584, 4096, 5120, 6144)


class Buf:
    __slots__ = ("name", "w", "r", "excl")

    def __init__(self, name="", excl=False):
        self.name = name
        self.w = None
        self.r = {}
        self.excl = excl


class Sched:
    ENG = ("pe", "act", "dve", "pool", "sp")
    NSLOT = 12

    def __init__(self, nc):
        self.nc = nc
        self.streams = {e: [] for e in self.ENG}
        self.cnt = {}
        self.seen = {e: {} for e in self.ENG}
        self.sems = {}
        self.slot_rr = {"sp": 0, "pool": 0}

    def alloc_sems(self, stack):
        for e in self.ENG:
            self.sems[e] = stack.enter_context(self.nc.semaphore("s_" + e))
            self.cnt[e] = 0
        for q in ("sp", "pool"):
            for i in range(self.NSLOT):
                k = "%s_d%d" % (q, i)
                self.sems[k] = stack.enter_context(self.nc.semaphore("s_" + k))
                self.cnt[k] = 0

    def _wait(self, eng, key, val):
        if val <= 0 or self.seen[eng].get(key, 0) >= val:
            return
        self.seen[eng][key] = val
        self.streams[eng].append(("wait", key, val))

    def _deps(self, eng, reads, writes):
        for b in reads:
            if b.w is not None and not (eng == "pe" and b.w[0] == "pe"):
                self._wait(eng, b.w[0], b.w[1])
            if b.excl:
                for k, v in b.r.items():
                    if k != eng:
                        self._wait(eng, k, v)
        for b in writes:
            if b.w is not None and not (eng == "pe" and b.w[0] == "pe"):
                self._wait(eng, b.w[0], b.w[1])
            for k, v in b.r.items():
                if not (eng == "pe" and k == "pe"):
                    self._wait(eng, k, v)

    def _mark(self, tok, reads, writes):
        for b in reads:
            b.r[tok[0]] = tok[1]
        for b in writes:
            b.w = tok
            b.r = {}

    def op(self, eng, fn, reads=(), writes=()):
        self._deps(eng, reads, writes)
        self.cnt[eng] += 1
        self.streams[eng].append(("op", fn, eng, 1))
        self._mark((eng, self.cnt[eng]), reads, writes)

    def dma(self, q, fn, reads=(), writes=()):
        self._deps(q, reads, writes)
        s = self.slot_rr[q]
        self.slot_rr[q] = (s + 1) % self.NSLOT
        key = "%s_d%d" % (q, s)
        self._wait(q, key, self.cnt[key])
        self.cnt[key] += 16
        self.streams[q].append(("op", fn, key, 16))
        self._mark((key, self.cnt[key]), reads, writes)

    def barrier(self):
        for e in self.ENG:
            for key, v in self.cnt.items():
                self._wait(e, key, v)

    def finish(self, eng="sp"):
        for key, v in self.cnt.items():
            self._wait(eng, key, v)

    def replay(self, block):
        sems = self.sems

        def run(name, handle):
            for it in self.streams[name]:
                if it[0] == "wait":
                    handle.wait_ge(sems[it[1]], it[2])
                else:
                    it[1](handle).then_inc(sems[it[2]], it[3])

        @block.tensor
        def _(e):
            run("pe", e)

        @block.scalar
        def _(e):
            run("act", e)

        @block.vector
        def _(e):
            run("dve", e)

        @block.gpsimd
        def _(e):
            run("pool", e)

        @block.sync
        def _(e):
            run("sp", e)


def seq(fns):
    def f(e):
        r = None
        for g in fns:
            r = g(e)
        return r
    return f


class _Stop(Exception):
    pass


def build_nc(NB, n_layers=DEPTH, stop=None):
    nc = bass.Bass("TRN2", target_bir_lowering=False)
    ckn = [0]

    def ck(tag=""):
        ckn[0] += 1
        if stop is not None and ckn[0] >= stop:
            print("STOP at checkpoint", ckn[0], tag, flush=True)
            raise _Stop()

    def din(name, shape):
        return nc.dram_tensor(name, list(shape), F32, kind="ExternalInput").ap()

    x_d = din("x", (NB, SEQ, D))
    ctx_d = din("ctx", (NB, CTX, D))
    cc_d = din("cc", (NB + 1, D))
    mod_w = din("mod_w", (DEPTH, D, 6 * D))
    mod_b = din("mod_b", (DEPTH, 6 * D))
    norm1_w = din("norm1_w", (DEPTH, D))
    norm2_w = din("norm2_w", (DEPTH, D))
    w_in = din("w_in", (DEPTH, D, D_IN))
    conv_a_w = din("conv_a_w", (DEPTH, 3, 512))
    rg_conv_w = din("rg_conv_w", (DEPTH, 2, 4, 512))
    rg_conv_b = din("rg_conv_b", (DEPTH, 2, 512))
    rg_wa = din("rg_wa", (DEPTH, 2, 8, 64, 64))
    rg_ba = din("rg_ba", (DEPTH, 2, 8, 64))
    rg_wx = din("rg_wx", (DEPTH, 2, 8, 64, 64))
    rg_bx = din("rg_bx", (DEPTH, 2, 8, 64))
    rg_lambda = din("rg_lambda", (DEPTH, 2, 512))
    da_lambda = din("da_lambda", (DEPTH, 4, 64))
    da_subln_w = din("da_subln_w", (DEPTH, 128))
    w_branch = din("w_branch", (DEPTH, 3, 512, D))
    w_out = din("w_out", (DEPTH, D, D))
    ffn_w13 = din("ffn_w13", (1, D, 2 * FF))
    ffn_w2 = din("ffn_w2", (1, FF, D))
    router_w = din("router_w", (1, D, NEXP))
    moe_w13 = din("moe_w13", (1, NEXP, D, 2 * FF))
    moe_w2 = din("moe_w2", (1, NEXP, FF, D))
    final_norm_w = din("final_norm_w", (D,))
    ident_d = din("ident", (128, 128))
    pmat_d = din("pmat", (128, 128))
    rope_d = din("rope", (2, 128, SEQ))
    out_d = nc.dram_tensor("out", [NB, SEQ, D], F32, kind="ExternalOutput").ap()
    kscr = nc.dram_tensor("kscr", [DEPTH, 4, 128, CTX], BF16, kind="Internal").ap()
    vscr = nc.dram_tensor("vscr", [DEPTH, 4, 128, 2, 128], BF16, kind="Internal").ap()

    S = Sched(nc)
    with contextlib.ExitStack() as st:
        S.alloc_sems(st)

        def sb(name, shape, dt):
            return st.enter_context(nc.sbuf_tensor("sb_" + name, list(shape), dt))

        xT = sb("xT", (128, 8, SEQ), F32)
        uT = sb("uT", (128, 8, SEQ), BF16)
        Y = sb("Y", (128, 24576), BF16)
        ft = [sb("ft%d" % i, (128, 512), F32) for i in range(5)]
        bt = [sb("bt%d" % i, (128, 512), BF16) for i in range(3)]
        PR = sb("PR", (128, 4160), BF16)
        wbuf = [sb("wb%d" % i, (128, 8, 128), BF16) for i in range(2)]
        NWB = 2
        hcar = sb("hcar", (128, 2), F32)
        ident = sb("ident", (128, 128), F32)
        onesb = sb("onesb", (128, 128), BF16)
        identb = sb("identb", (128, 128), BF16)
        pmat = sb("pmat", (128, 128), BF16)
        rgW = sb("rgW", (128, 4, 128), BF16)
        prow = ft[1][:, 0:128]
        pcolA = sb("pcolA", (128, DEPTH, 96), F32)
        pcolB = sb("pcolB", (128, 112), F32)
        ccol = sb("ccol", (128, 8 * (NB + 1)), F32)
        scol = sb("scol", (128, 8, NB + 1), BF16)
        modc = sb("modc", (128, DEPTH, NB + 1, 48), F32)
        wm = sb("wm", (128, 16), F32)
        lamt = ft[2][:, 0:256]
        lamc = sb("lamc", (128, DEPTH, 4), F32)
        cst = sb("cst", (128, 4), F32)
        Kc = PR[:, 0:CTX]
        Vc = PR[:, CTX:2 * CTX].rearrange("p (t d) -> p t d", d=128)
        hsave = sb("hsave", (128, DEPTH, 4, 2), F32)
        rt = sb("rt", (128, 48), F32)
        ps = [st.enter_context(nc.psum_tensor("ps%d" % i, [128, 512], F32)) for i in range(8)]
        PB = [Buf("ps%d" % i, excl=True) for i in range(8)]
        _rem = nc.sbuf_bytes_remaining
        assert 212863 - _rem <= 179500, ("SBUF over the safe limit", 212863 - _rem)

        yA = Y[:, 0:8192].rearrange("p (j t) -> p j t", j=4)
        yR = Y[:, 8192:16384].rearrange("p (j t) -> p j t", j=4)
        yD = Y[:, 16384:24576].rearrange("p (j t) -> p j t", j=4)
        ropec = Y[:, 0:2048]
        ropes = Y[:, 2048:4096]
        qT = Y[:, 4096:6144]
        kT = Y[:, 6144:8192]
        vtok = Y[:, 8192:10240].rearrange("p (c d) -> p c d", d=128)
        qT2 = Y[:, 10240:12288]
        def ffw(i):
            base = i * 12288
            return (Y[:, base:base + 4096].rearrange("p (k n) -> p k n", k=8),
                    Y[:, base + 4096:base + 8192].rearrange("p (k n) -> p k n", k=8),
                    Y[:, base + 8192:base + 12288].rearrange("p (f n) -> p f n", f=4))
        def modst(i):
            return Y[:, i * 4096:(i + 1) * 4096].rearrange("p (k n) -> p k n", k=8)
        iost = Y[:, 16384:24576].bitcast(F32).rearrange("p (t d) -> p t d", t=4)
        WTt = sb("WTt", (128, 16, 8), F32)

        B = {}

        def gb(name):
            if name not in B:
                B[name] = Buf(name)
            return B[name]

        bank_rr = [0]

        def bank(lo=0, hi=8):
            i = lo + bank_rr[0] % (hi - lo)
            bank_rr[0] += 1
            return i

        wb_rr = [0]

        def ACT(out, in_, func, reads, writes, scale=None, bias=None):
            kw = {}
            if scale is not None:
                kw["scale"] = scale
            if bias is not None:
                kw["bias"] = bias
            S.op("act", lambda e: e.activation(out=out, in_=in_, func=func, **kw), reads, writes)

        def TT(out, a, b, op, reads, writes):
            S.op("dve", lambda e: e.tensor_tensor(out=out, in0=a, in1=b, op=op), reads, writes)

        def STT(out, in0, scalar, in1, op0, op1, reads, writes):
            S.op("dve", lambda e: e.scalar_tensor_tensor(out=out, in0=in0, scalar=scalar, in1=in1, op0=op0, op1=op1), reads, writes)

        def TS(out, in0, s1, s2, op0, op1, reads, writes):
            if s2 is None:
                S.op("dve", lambda e: e.tensor_scalar(out=out, in0=in0, scalar1=s1, scalar2=None, op0=op0), reads, writes)
            else:
                S.op("dve", lambda e: e.tensor_scalar(out=out, in0=in0, scalar1=s1, scalar2=s2, op0=op0, op1=op1), reads, writes)

        def RECIP(out, in_, reads, writes):
            S.op("dve", lambda e: e.reciprocal(out=out, in_=in_), reads, writes)

        def COPYV(out, in_, reads, writes):
            S.op("dve", lambda e: e.tensor_copy(out=out, in_=in_), reads, writes)

        def MM(items, reads, writes):
            fns = [(lambda e, o=o, l=l, r=r, a=a, z=z: e.matmul(o, l, r, start=a, stop=z)) for (o, l, r, a, z) in items]
            S.op("pe", seq(fns), reads, writes)

        def TR(out, in_, reads, writes):
            S.op("pe", lambda e: e.transpose(out, in_, ident[:]), list(reads) + [gb("ident")], writes)

        def DMAW(out, in_, reads, writes):
            S.dma("pool", lambda e: e.dma_start(out=out, in_=in_), reads, writes)

        def DMAS(out, in_, reads, writes):
            S.dma("sp", lambda e: e.dma_start(out=out, in_=in_), reads, writes)

        DMAS(ident[:], ident_d[:, :], [], [gb("ident")])
        DMAW(pmat[:], pmat_d[:, :], [], [gb("pmat")])
        S.op("dve", lambda e: e.memset(onesb[:], 1.0), [], [gb("onesb")])
        COPYV(identb[:], ident[:], [gb("ident")], [gb("identb")])
        S.op("dve", lambda e: e.memset(cst[:, 0:1], EPS), [], [gb("cst")])
        S.op("dve", lambda e: e.memset(cst[:, 1:2], 1.0), [], [gb("cst")])
        S.op("dve", lambda e: e.memset(cst[:, 2:3], 0.0), [], [gb("cst")])
        S.op("dve", lambda e: e.memset(rgW[:], 0.0), [], [gb("rgW")])
        S.op("dve", lambda e: e.memset(prow, 0.0), [], [gb("prow")])

        def rows_to_cols(row_specs, nrows, dst):
            for ap, r0, r in row_specs:
                DMAS(prow[r0:r0 + r, :], ap, [], [gb("prow")])
            pb = bank()
            TR(ps[pb][:, 0:128], prow, [gb("prow")], [PB[pb]])
            COPYV(dst, ps[pb][:, 0:nrows], [PB[pb]], [gb("pcol")])

        for l in range(DEPTH):
            specs = [
                (norm1_w[l].rearrange("(r p) -> r p", p=128), 0, 8),
                (norm2_w[l].rearrange("(r p) -> r p", p=128), 8, 8),
                (conv_a_w[l].rearrange("k (j p) -> (k j) p", p=128), 16, 12),
                (rg_conv_w[l].rearrange("d k (j p) -> (d k j) p", p=128), 28, 32),
                (rg_conv_b[l].rearrange("d (j p) -> (d j) p", p=128), 60, 8),
                (rg_ba[l].rearrange("d (j g) i -> (d j) (g i)", g=2), 68, 8),
                (rg_bx[l].rearrange("d (j g) i -> (d j) (g i)", g=2), 76, 8),
                (rg_lambda[l].rearrange("d (j p) -> (d j) p", p=128), 84, 8),
            ]
            rows_to_cols(specs, 92, pcolA[:, l, 0:92])
            ACT(pcolA[:, l, 84:92], pcolA[:, l, 84:92], AF.Exp, [gb("pcol")], [gb("pcol")], scale=-1.0)
            ACT(pcolA[:, l, 84:92], pcolA[:, l, 84:92], AF.Ln, [gb("pcol")], [gb("pcol")], scale=1.0, bias=cst[:, 1:2])
            TS(pcolA[:, l, 84:92], pcolA[:, l, 84:92], -8.0, None, ALU.mult, None, [gb("pcol")], [gb("pcol")])
        specs = [
            (mod_b[0].rearrange("(r p) -> r p", p=128), 0, 48),
            (mod_b[1].rearrange("(r p) -> r p", p=128), 48, 48),
            (final_norm_w.rearrange("(r p) -> r p", p=128), 96, 8),
            (da_subln_w[0].rearrange("(r p) -> r p", p=128), 104, 1),
            (da_subln_w[1].rearrange("(r p) -> r p", p=128), 105, 1),
        ]
        rows_to_cols(specs, 106, pcolB[:, 0:106])
        lam_init = [0.8 - 0.6 * math.exp(-0.3 * l) for l in range(DEPTH)]
        for l in range(DEPTH):
            TS(pcolB[:, 104 + l:105 + l], pcolB[:, 104 + l:105 + l], 1.0 - lam_init[l], None, ALU.mult, None, [gb("pcol")], [gb("pcol")])
        rows_to_cols([(cc_d.rearrange("b (r p) -> (b r) p", p=128), 0, 8 * (NB + 1))], 8 * (NB + 1), ccol[:, :])
        ACT(scol[:].rearrange("p k b -> p b k"), ccol[:].rearrange("p (b k) -> p b k", k=8), AF.Silu, [gb("pcol")], [gb("scol")])
        for l in range(DEPTH):
            DMAS(lamt, da_lambda[l].rearrange("a d -> (a d)").partition_broadcast(128), [], [gb("lamt")])
            TT(lamt[:, 0:64], lamt[:, 0:64], lamt[:, 64:128], ALU.mult, [gb("lamt")], [gb("lamt")])
            TT(lamt[:, 128:192], lamt[:, 128:192], lamt[:, 192:256], ALU.mult, [gb("lamt")], [gb("lamt")])
            S.op("dve", lambda e, l=l: e.reduce_sum(out=lamc[:, l, 1:2], in_=lamt[:, 0:64], axis=AX.X), [gb("lamt")], [gb("lamc")])
            S.op("dve", lambda e, l=l: e.reduce_sum(out=lamc[:, l, 2:3], in_=lamt[:, 128:192], axis=AX.X), [gb("lamt")], [gb("lamc")])
            ACT(lamc[:, l, 1:3], lamc[:, l, 1:3], AF.Exp, [gb("lamc")], [gb("lamc")])
            STT(lamc[:, l, 0:1], lamc[:, l, 2:3], -lam_init[l], lamc[:, l, 1:2], ALU.add, ALU.subtract, [gb("lamc")], [gb("lamc")])

        for l in range(DEPTH):
            pbm = bank()
            for g in range(12):
                stg = modst(g % 2)
                sbuf_b = gb("modst%d" % (g % 2))
                DMAW(stg, mod_w[l][:, g * 512:(g + 1) * 512].rearrange("(k p) n -> p k n", p=128), [], [sbuf_b])
                items = []
                for oc in range(4):
                    occ = g * 4 + oc
                    for kk in range(8):
                        items.append((ps[pbm][:, occ * (NB + 1):(occ + 1) * (NB + 1)],
                                      stg[:, kk, oc * 128:(oc + 1) * 128], scol[:, kk, :], kk == 0, kk == 7))
                MM(items, [sbuf_b, gb("scol")], [PB[pbm]])
            for r in range(NB + 1):
                TT(modc[:, l, r, :], ps[pbm][:, 0:48 * (NB + 1)].rearrange("p (o r) -> p r o", r=NB + 1)[:, r, :],
                   pcolB[:, l * 48:(l + 1) * 48], ALU.add, [PB[pbm], gb("pcol")], [gb("modc")])
        S.barrier()
        try:
            ck("setup")
        except _Stop:
            stop = -1

        def load_w(src_cols_ap):
            i = wb_rr[0] % NWB
            wb_rr[0] += 1
            DMAW(wbuf[i][:], src_cols_ap.rearrange("(k p) n -> p k n", p=128), [], [gb("wb%d" % i)])
            return wbuf[i], gb("wb%d" % i)

        def proj(w, wB, rhs_k, n, reads, lo=0, hi=8):
            pb = bank(lo, hi)
            MM([(ps[pb][:, 0:n], w[:, kk, :], rhs_k(kk), kk == 0, kk == 7) for kk in range(8)],
               [wB] + list(reads), [PB[pb]])
            return pb

        def norm_mod(T, blocks, N, col_w, sh_ap, dst_bf, XB, UB, f32_tap=None):
            for bi in blocks:
                t0 = bi * N
                pb = bank()
                items = []
                for c in range(8):
                    sq = bt[c % 2]
                    sqB = gb("bt%d" % (c % 2))
                    ACT(sq[:, 0:N], xT[:, c, t0:t0 + N], AF.Square, [XB[bi]], [sqB])
                    MM([(ps[pb][:, 0:N], onesb[:], sq[:, 0:N], c == 0, c == 7)], [sqB, gb("onesb")], [PB[pb]])
                rstd = ft[4]
                ACT(rstd[:, 0:N], ps[pb][:, 0:N], AF.Sqrt, [PB[pb], gb("cst")], [gb("ft4")], scale=1.0 / D, bias=cst[:, 0:1])
                RECIP(rstd[:, 0:N], rstd[:, 0:N], [gb("ft4")], [gb("ft4")])
                for c in range(8):
                    tmp = ft[c % 2]
                    tB = gb("ft%d" % (c % 2))
                    TT(tmp[:, 0:N], xT[:, c, t0:t0 + N], rstd[:, 0:N], ALU.mult, [XB[bi], gb("ft4")], [tB])
                    if f32_tap is not None:
                        ACT(tmp[:, 0:N], tmp[:, 0:N], AF.Identity, [tB, gb("wm"), gb("modc")], [tB],
                            scale=col_w[:, c:c + 1], bias=sh_ap[:, c:c + 1])
                        f32_tap(bi, c, tmp, tB)
                        COPYV(dst_bf[:, c, t0:t0 + N], tmp[:, 0:N], [tB], [UB[bi]])
                    else:
                        ACT(dst_bf[:, c, t0:t0 + N], tmp[:, 0:N], AF.Identity, [tB, gb("wm"), gb("modc")], [UB[bi]],
                            scale=col_w[:, c:c + 1], bias=sh_ap[:, c:c + 1])

        def run_pass(b, is_ctx):
            T = CTX if is_ctx else SEQ
            N = min(512, T)
            nblk = T // N
            ntt = N // 128
            r = NB if is_ctx else b
            XB = [gb("X%d" % i) for i in range(nblk)]
            UB = [gb("U%d" % i) for i in range(nblk)]
            YAB = [gb("YA%d" % i) for i in range(nblk)]
            YRB = [gb("YR%d" % i) for i in range(nblk)]
            YDB = [gb("YD%d" % i) for i in range(nblk)]
            src = ctx_d[b] if is_ctx else x_d[b]

            for bi in range(nblk):
                DMAS(iost[:, 0:ntt, :], src[bi * N:(bi + 1) * N, :].rearrange("(t p) d -> p t d", p=128), [], [gb("iost")])
                for c in range(8):
                    pb = bank()
                    for tt in range(ntt):
                        TR(ps[pb][:, tt * 128:(tt + 1) * 128], iost[:, tt, c * 128:(c + 1) * 128], [gb("iost")], [PB[pb]])
                    if c % 2 == 0:
                        ACT(xT[:, c, bi * N:(bi + 1) * N], ps[pb][:, 0:N], AF.Copy, [PB[pb]], [XB[bi]])
                    else:
                        COPYV(xT[:, c, bi * N:(bi + 1) * N], ps[pb][:, 0:N], [PB[pb]], [XB[bi]])

            ck("load")
            for l in range(n_layers):
                partial = is_ctx and (l == DEPTH - 1)
                pc = pcolA[:, l, :]
                mc = modc[:, l, r, :]
                STT(wm[:, 0:8], mc[:, 8:16], 1.0, pc[:, 0:8], ALU.add, ALU.mult, [gb("modc"), gb("pcol")], [gb("wm")])
                STT(wm[:, 8:16], mc[:, 32:40], 1.0, pc[:, 8:16], ALU.add, ALU.mult, [gb("modc"), gb("pcol")], [gb("wm")])
                norm_mod(T, range(nblk), N, wm[:, 0:8], mc[:, 0:8], uT, XB, UB)

                ck("norm1")

                def u_k(bi):
                    return lambda kk: uT[:, kk, bi * N:(bi + 1) * N]


                ck("rgW")
                if not is_ctx:
                    DMAW(ropec, rope_d[0], [], [gb("rope")])
                    DMAW(ropes, rope_d[1], [], [gb("rope")])
                S.op("dve", lambda e, T=T: e.memset(qT[64:128, 0:T], 0.0), [], [gb("qT")])
                S.op("dve", lambda e, T=T: e.memset(qT2[0:64, 0:T], 0.0), [], [gb("qT")])
                for h in range(4):
                    for which, c0, dstT, dB in ((0, C_K, kT, gb("kT")), (1, C_Q, qT, gb("qT"))):
                        if partial and which == 1:
                            continue
                        w, wB = load_w(w_in[l][:, c0 + h * 128:c0 + (h + 1) * 128])
                        if h == 0 and which == 0:
                            ck("kload")
                        for bi in range(nblk):
                            pb = proj(w, wB, u_k(bi), N, [UB[bi]], 0, 4)
                            sl = slice(bi * N, (bi + 1) * N)
                            if h == 0 and which == 0 and bi == 0:
                                ck("kproj")
                            if is_ctx:
                                if which == 0:
                                    ACT(dstT[:, sl], ps[pb][:, 0:N], AF.Copy, [PB[pb]], [dB])
                                    if h == 0:
                                        ck("kact")
                                    DMAS(kscr[l, h], dstT[:, sl], [dB], [gb("kscr")])
                                    if h == 0:
                                        ck("ksave")
                                else:
                                    ACT(qT[0:64, sl], ps[pb][0:64, 0:N], AF.Copy, [PB[pb]], [dB])
                                    ACT(qT2[64:128, sl], ps[pb][64:128, 0:N], AF.Copy, [PB[pb]], [dB])
                            else:
                                raw = bt[2]
                                rB = gb("bt2")
                                ACT(raw[:, 0:N], ps[pb][:, 0:N], AF.Copy, [PB[pb]], [rB])
                                pb2 = bank(0, 4)
                                MM([(ps[pb2][:, 0:N], pmat[:], raw[:, 0:N], True, True)], [rB, gb("pmat")], [PB[pb2]])
                                if h == 0 and which == 0:
                                    ck("ropemm%d" % bi)
                                t1 = ft[2]
                                t2 = ft[3]
                                ACT(t1[:, 0:N], ps[pb][:, 0:N], AF.Copy, [PB[pb]], [gb("ft2")])
                                TT(t1[:, 0:N], t1[:, 0:N], ropec[:, sl], ALU.mult, [gb("ft2"), gb("rope")], [gb("ft2")])
                                if h == 0 and which == 0:
                                    ck("ropet1%d" % bi)
                                ACT(t2[:, 0:N], ps[pb2][:, 0:N], AF.Copy, [PB[pb2]], [gb("ft3")])
                                TT(t2[:, 0:N], t2[:, 0:N], ropes[:, sl], ALU.mult, [gb("ft3"), gb("rope")], [gb("ft3")])
                                if which == 0:
                                    TT(dstT[:, sl], t1[:, 0:N], t2[:, 0:N], ALU.add, [gb("ft2"), gb("ft3")], [dB])
                                else:
                                    TT(qT[0:64, sl], t1[0:64, 0:N], t2[0:64, 0:N], ALU.add, [gb("ft2"), gb("ft3")], [dB])
                                    TT(qT2[64:128, sl], t1[64:128, 0:N], t2[64:128, 0:N], ALU.add, [gb("ft2"), gb("ft3")], [dB])
                            if h == 0 and which == 0 and not is_ctx:
                                ck("kblk%d" % bi)
                        if h == 0 and which == 0 and not is_ctx:
                            ck("kdone")
                    if h == 0:
                        ck("kq")
                    w, wB = load_w(w_in[l][:, C_V + h * 128:C_V + (h + 1) * 128])
                    for tg in range(T // 512 if T >= 512 else 1):
                        pb = bank(0, 4)
                        nt = min(4, T // 128)
                        items = []
                        for tt in range(nt):
                            tok0 = (tg * 4 + tt) * 128
                            for kk in range(8):
                                items.append((ps[pb][:, tt * 128:(tt + 1) * 128], uT[:, kk, tok0:tok0 + 128], w[:, kk, :], kk == 0, kk == 7))
                        MM(items, [wB] + UB, [PB[pb]])
                        ACT(vtok[:, tg * 4:tg * 4 + nt, :], ps[pb][:, 0:nt * 128].rearrange("p (t d) -> p t d", d=128), AF.Copy, [PB[pb]], [gb("vtok")])
                        if is_ctx:
                            DMAS(vscr[l, h], vtok[:, 0:2, :], [gb("vtok")], [gb("vscr")])
                    if h == 0:
                        ck("v")
                    if partial:
                        continue
                    keys = []
                    if not is_ctx:
                        DMAS(Kc, kscr[l, h], [gb("kscr")], [gb("Kc")])
                        DMAS(Vc, vscr[l, h], [gb("vscr")], [gb("Vc")])
                        for kc in range(2):
                            keys.append((Kc[:, kc * 128:(kc + 1) * 128], Vc[:, kc, :], [gb("Kc"), gb("Vc")]))
                    for kc in range(T // 128):
                        keys.append((kT[:, kc * 128:(kc + 1) * 128], vtok[:, kc, :], [gb("kT"), gb("vtok")]))
                    nk = len(keys)
                    for bi in range(nblk):
                        sl = slice(bi * N, (bi + 1) * N)
                        for ki, (kap, vap, kB) in enumerate(keys):
                            for m in range(2):
                                pbs = m * 2 + (ki % 2)
                                qsrc = qT if m == 0 else qT2
                                MM([(ps[pbs][:, 0:N], kap, qsrc[:, sl], True, True)], kB + [gb("qT")], [PB[pbs]])
                                P = bt[m]
                                PBf = gb("bt%d" % m)
                                ACT(P[:, 0:N], ps[pbs][:, 0:N], AF.Exp, [PB[pbs]], [PBf], scale=0.125)
                                MM([(ps[4 + 2 * m][:, 0:N], vap, P[:, 0:N], ki == 0, ki == nk - 1)],
                                   kB + [PBf], [PB[4 + 2 * m]])
                                accB = gb("ft%d" % (2 + m))
                                if ki == 0:
                                    COPYV(ft[2 + m][:, 0:N], P[:, 0:N], [PBf], [accB])
                                else:
                                    TT(ft[2 + m][:, 0:N], ft[2 + m][:, 0:N], P[:, 0:N], ALU.add, [accB, PBf], [accB])
                        for m in range(2):
                            accB = gb("ft%d" % (2 + m))
                            ACT(bt[m][:, 0:N], ft[2 + m][:, 0:N], AF.Copy, [accB], [gb("bt%d" % m)])
                            MM([(ps[5 + 2 * m][:, 0:N], onesb[:], bt[m][:, 0:N], True, True)], [gb("bt%d" % m), gb("onesb")], [PB[5 + 2 * m]])
                        if h == 0 and bi == 0:
                            ck("scores")
                        RECIP(ft[0][:, 0:N], ps[5][:, 0:N], [PB[5]], [gb("ft0")])
                        TT(ft[0][:, 0:N], ps[4][:, 0:N], ft[0][:, 0:N], ALU.mult, [PB[4], gb("ft0")], [gb("ft0")])
                        RECIP(ft[1][:, 0:N], ps[7][:, 0:N], [PB[7]], [gb("ft1")])
                        TT(ft[1][:, 0:N], ps[6][:, 0:N], ft[1][:, 0:N], ALU.mult, [PB[6], gb("ft1")], [gb("ft1")])
                        STT(ft[0][:, 0:N], ft[1][:, 0:N], lamc[:, l, 0:1], ft[0][:, 0:N], ALU.mult, ALU.add, [gb("ft1"), gb("ft0"), gb("lamc")], [gb("ft0")])
                        ACT(bt[2][:, 0:N], ft[0][:, 0:N], AF.Square, [gb("ft0")], [gb("bt2")])
                        MM([(ps[0][:, 0:N], onesb[:], bt[2][:, 0:N], True, True)], [gb("bt2"), gb("onesb")], [PB[0]])
                        ACT(ft[1][:, 0:N], ps[0][:, 0:N], AF.Sqrt, [PB[0], gb("cst")], [gb("ft1")], scale=1.0 / 128, bias=cst[:, 0:1])
                        RECIP(ft[1][:, 0:N], ft[1][:, 0:N], [gb("ft1")], [gb("ft1")])
                        TT(ft[0][:, 0:N], ft[0][:, 0:N], ft[1][:, 0:N], ALU.mult, [gb("ft0"), gb("ft1")], [gb("ft0")])
                        ACT(yD[:, h, sl], ft[0][:, 0:N], AF.Identity, [gb("ft0"), gb("pcol")], [YDB[bi]], scale=pcolB[:, 104 + l:105 + l])
                S.barrier()
                ck("attn")

                if not partial:
                    cx = PR[:, 0:T + 2]
                    S.op("dve", lambda e: e.memset(PR[:, 0:1], 0.0), [], [gb("cx")])
                    S.op("dve", lambda e, T=T: e.memset(PR[:, T + 1:T + 2], 0.0), [], [gb("cx")])
                    for j in range(4):
                        acst = PR[:, 2112:2112 + T]
                        wc, wcB = load_w(w_in[l][:, C_AC + j * 128:C_AC + (j + 1) * 128])
                        for bi in range(nblk):
                            pbc = proj(wc, wcB, u_k(bi), N, [UB[bi]])
                            ACT(acst[:, bi * N:(bi + 1) * N], ps[pbc][:, 0:N], AF.Copy, [PB[pbc]], [gb("acst")])
                        wx_, wxB = load_w(w_in[l][:, C_AX + j * 128:C_AX + (j + 1) * 128])
                        for bi in range(nblk):
                            pbx = proj(wx_, wxB, u_k(bi), N, [UB[bi]])
                            TT(PR[:, 1 + bi * N:1 + (bi + 1) * N], ps[pbx][:, 0:N], acst[:, bi * N:(bi + 1) * N], ALU.mult, [PB[pbx], gb("acst")], [gb("cx")])
                        wb_, wbB = load_w(w_in[l][:, C_AB + j * 128:C_AB + (j + 1) * 128])
                        for bi in range(nblk):
                            pbb = proj(wb_, wbB, u_k(bi), N, [UB[bi]])
                            t = ft[1]
                            o = bi * N
                            TS(t[:, 0:N], PR[:, o:o + N], pc[:, 16 + j:17 + j], None, ALU.mult, None, [gb("cx"), gb("pcol")], [gb("ft1")])
                            STT(t[:, 0:N], PR[:, o + 1:o + 1 + N], pc[:, 20 + j:21 + j], t[:, 0:N], ALU.mult, ALU.add, [gb("cx"), gb("pcol"), gb("ft1")], [gb("ft1")])
                            STT(t[:, 0:N], PR[:, o + 2:o + 2 + N], pc[:, 24 + j:25 + j], t[:, 0:N], ALU.mult, ALU.add, [gb("cx"), gb("pcol"), gb("ft1")], [gb("ft1")])
                            TT(yA[:, j, o:o + N], ps[pbb][:, 0:N], t[:, 0:N], ALU.mult, [PB[pbb], gb("ft1")], [YAB[bi]])
                    S.barrier()
                    ck("mixA")

                xr = PR[:, 0:T + 6]
                hF = PR[:, 2112:2112 + T]
                for j in range(4):
                    S.op("dve", lambda e: e.memset(PR[:, 0:3], 0.0), [], [gb("xr")])
                    S.op("dve", lambda e, T=T: e.memset(PR[:, T + 3:T + 6], 0.0), [], [gb("xr")])
                    for which, wsrc in enumerate((rg_wa, rg_wx)):
                        for d in range(2):
                            for g in range(2):
                                DMAW(rgW[g * 64:(g + 1) * 64, which * 2 + d, g * 64:(g + 1) * 64], wsrc[l, d, 2 * j + g], [], [gb("rgW")])
                    w, wB = load_w(w_in[l][:, C_RX + j * 128:C_RX + (j + 1) * 128])
                    for bi in range(nblk):
                        pb = proj(w, wB, u_k(bi), N, [UB[bi]])
                        ACT(PR[:, 3 + bi * N:3 + (bi + 1) * N], ps[pb][:, 0:N], AF.Copy, [PB[pb]], [gb("xr")])
                    if not partial:
                        wg, wgB = load_w(w_in[l][:, C_RG + j * 128:C_RG + (j + 1) * 128])
                    for d in range(2):
                        order = list(range(nblk)) if d == 0 else list(range(nblk - 1, -1, -1))
                        prev_h = None
                        for oi, bi in enumerate(order):
                            o = bi * N + (0 if d == 0 else 3)
                            xc = bt[2]
                            cw = 28 + d * 16
                            TS(ft[0][:, 0:N], PR[:, o:o + N], pc[:, cw + j:cw + j + 1], pc[:, 60 + d * 4 + j:61 + d * 4 + j], ALU.mult, ALU.add, [gb("xr"), gb("pcol")], [gb("ft0")])
                            for k in range(1, 4):
                                dst = xc[:, 0:N] if k == 3 else ft[0][:, 0:N]
                                dB = gb("bt2") if k == 3 else gb("ft0")
                                STT(dst, PR[:, o + k:o + k + N], pc[:, cw + 4 * k + j:cw + 4 * k + j + 1], ft[0][:, 0:N], ALU.mult, ALU.add, [gb("xr"), gb("pcol"), gb("ft0")], [dB])
                            pa = bank()
                            pi = bank()
                            MM([(ps[pa][:, 0:N], rgW[:, d, :], xc[:, 0:N], True, True)], [gb("rgW"), gb("bt2")], [PB[pa]])
                            MM([(ps[pi][:, 0:N], rgW[:, 2 + d, :], xc[:, 0:N], True, True)], [gb("rgW"), gb("bt2")], [PB[pi]])
                            ACT(ft[1][:, 0:N], ps[pa][:, 0:N], AF.Sigmoid, [PB[pa], gb("pcol")], [gb("ft1")], scale=1.0, bias=pc[:, 68 + d * 4 + j:69 + d * 4 + j])
                            ACT(ft[2][:, 0:N], ps[pi][:, 0:N], AF.Sigmoid, [PB[pi], gb("pcol")], [gb("ft2")], scale=1.0, bias=pc[:, 76 + d * 4 + j:77 + d * 4 + j])
                            ACT(ft[1][:, 0:N], ft[1][:, 0:N], AF.Exp, [gb("ft1"), gb("pcol")], [gb("ft1")], scale=pc[:, 84 + d * 4 + j:85 + d * 4 + j])
                            ACT(ft[3][:, 0:N], ft[1][:, 0:N], AF.Square, [gb("ft1")], [gb("ft3")])
                            ACT(ft[3][:, 0:N], ft[3][:, 0:N], AF.Sqrt, [gb("ft3"), gb("cst")], [gb("ft3")], scale=-1.0, bias=cst[:, 1:2])
                            TT(ft[2][:, 0:N], ft[2][:, 0:N], xc[:, 0:N], ALU.mult, [gb("ft2"), gb("bt2")], [gb("ft2")])
                            TT(ft[2][:, 0:N], ft[2][:, 0:N], ft[3][:, 0:N], ALU.mult, [gb("ft2"), gb("ft3")], [gb("ft2")])
                            hcur = ft[4]
                            hB = gb("ft4")
                            if prev_h is None:
                                init = cst[:, 2:3] if is_ctx else hsave[:, l, j, d:d + 1]
                                iB = [gb("cst"), gb("hsave")]
                            else:
                                COPYV(hcar[:, d:d + 1], hcur[:, N - 1:N] if d == 0 else hcur[:, 0:1], [hB], [gb("hcar")])
                                init = hcar[:, d:d + 1]
                                iB = [gb("hcar")]
                            if d == 0:
                                S.op("dve", lambda e, hcur=hcur, init=init, N=N: e.tensor_tensor_scan(out=hcur[:, 0:N], data0=ft[1][:, 0:N], data1=ft[2][:, 0:N], initial=init, op0=ALU.mult, op1=ALU.add),
                                     [gb("ft1"), gb("ft2")] + iB, [hB])
                                if not partial:
                                    ACT(hF[:, bi * N:(bi + 1) * N], hcur[:, 0:N], AF.Copy, [hB], [gb("hF")])
                            else:
                                S.op("dve", lambda e, hcur=hcur, init=init, N=N: e.tensor_tensor_scan(out=hcur[:, N - 1::-1] if False else hcur[:, 0:N][:, ::-1], data0=ft[1][:, 0:N][:, ::-1], data1=ft[2][:, 0:N][:, ::-1], initial=init, op0=ALU.mult, op1=ALU.add),
                                     [gb("ft1"), gb("ft2")] + iB, [hB])
                                if not partial:
                                    pg = proj(wg, wgB, u_k(bi), N, [UB[bi]])
                                    ACT(ft[3][:, 0:N], ps[pg][:, 0:N], AF.Gelu_apprx_tanh, [PB[pg]], [gb("ft3")])
                                    TT(ft[0][:, 0:N], hcur[:, 0:N], hF[:, bi * N:(bi + 1) * N], ALU.add, [hB, gb("hF")], [gb("ft0")])
                                    TT(yR[:, j, bi * N:(bi + 1) * N], ft[0][:, 0:N], ft[3][:, 0:N], ALU.mult, [gb("ft0"), gb("ft3")], [YRB[bi]])
                            prev_h = (hcur, hB)
                        if is_ctx:
                            last = prev_h[0][:, N - 1:N] if d == 0 else prev_h[0][:, 0:1]
                            COPYV(hsave[:, l, j, d:d + 1], last, [prev_h[1]], [gb("hsave")])
                S.barrier()
                ck("mixB")
                if partial:
                    continue

                mT = PR[:, 0:4096].rearrange("p (c n) -> p c n", c=8)
                for bi in range(nblk):
                    sl = slice(bi * N, (bi + 1) * N)
                    for c in range(8):
                        for br, (c0, ysrc, yB) in enumerate(((C_GA, yA, YAB), (C_GR, yR, YRB), (C_GD, yD, YDB))):
                            w, wB = load_w(w_in[l][:, c0 + c * 128:c0 + (c + 1) * 128])
                            pg = proj(w, wB, u_k(bi), N, [UB[bi]])
                            i = wb_rr[0] % NWB
                            wb_rr[0] += 1
                            DMAW(wbuf[i][:, 0:4, :], w_branch[l, br][:, c * 128:(c + 1) * 128].rearrange("(k p) n -> p k n", p=128), [], [gb("wb%d" % i)])
                            pbr = bank()
                            MM([(ps[pbr][:, 0:N], wbuf[i][:, kk, :], ysrc[:, kk, sl], kk == 0, kk == 3) for kk in range(4)],
                               [gb("wb%d" % i), yB[bi]], [PB[pbr]])
                            sg = ft[br % 2]
                            sgB = gb("ft%d" % (br % 2))
                            ACT(sg[:, 0:N], ps[pg][:, 0:N], AF.Sigmoid, [PB[pg]], [sgB])
                            if br == 0:
                                TT(ft[2][:, 0:N], ps[pbr][:, 0:N], sg[:, 0:N], ALU.mult, [PB[pbr], sgB], [gb("ft2")])
                            else:
                                TT(ft[3][:, 0:N], ps[pbr][:, 0:N], sg[:, 0:N], ALU.mult, [PB[pbr], sgB], [gb("ft3")])
                                dst = mT[:, c, 0:N] if br == 2 else ft[2][:, 0:N]
                                dB = gb("mT") if br == 2 else gb("ft2")
                                TT(dst, ft[2][:, 0:N], ft[3][:, 0:N], ALU.add, [gb("ft2"), gb("ft3")], [dB])
                    for c2 in range(8):
                        w, wB = load_w(w_out[l][:, c2 * 128:(c2 + 1) * 128])
                        py = proj(w, wB, lambda kk: mT[:, kk, 0:N], N, [gb("mT")])
                        STT(xT[:, c2, sl], ps[py][:, 0:N], mc[:, 16 + c2:17 + c2], xT[:, c2, sl], ALU.mult, ALU.add, [PB[py], gb("modc"), XB[bi]], [XB[bi]])
                S.barrier()
                ck("merge")

                is_moe = (l % 2 == 1)
                hT = uT
                if is_moe:
                    rw = ft[3]
                    DMAS(rw[:, 0:64].rearrange("p (k e) -> p k e", e=8), router_w[0].rearrange("(k p) e -> p k e", p=128), [], [gb("ft3")])
                    WT = WTt[:]
                    pr_bank = [None]

                    def tap(bi, c, tmp, tB):
                        if c == 0:
                            pr_bank[0] = [bank() for _ in range(ntt)]
                        pbs_ = pr_bank[0]
                        MM([(ps[pbs_[tt]][:, 0:8], tmp[:, tt * 128:(tt + 1) * 128], rw[:, c * 8:(c + 1) * 8], c == 0, c == 7) for tt in range(ntt)],
                           [tB, gb("ft3")], [PB[p_] for p_ in pbs_])
                        if c == 7:
                            for tt in range(ntt):
                                gt = bi * ntt + tt
                                L = rt[:, 0:8]
                                pb = pbs_[tt]
                                COPYV(L, ps[pb][:, 0:8], [PB[pb]], [gb("rt")])
                                S.op("dve", lambda e: e.reduce_max(out=rt[:, 40:41], in_=rt[:, 0:8], axis=AX.X), [gb("rt")], [gb("rt")])
                                TS(rt[:, 8:16], rt[:, 0:8], rt[:, 40:41], None, ALU.is_equal, None, [gb("rt")], [gb("rt")])
                                STT(rt[:, 16:24], rt[:, 8:16], -1e30, rt[:, 0:8], ALU.mult, ALU.add, [gb("rt")], [gb("rt")])
                                S.op("dve", lambda e: e.reduce_max(out=rt[:, 41:42], in_=rt[:, 16:24], axis=AX.X), [gb("rt")], [gb("rt")])
                                TS(rt[:, 24:32], rt[:, 16:24], rt[:, 41:42], None, ALU.is_equal, None, [gb("rt")], [gb("rt")])
                                TT(rt[:, 42:43], rt[:, 41:42], rt[:, 40:41], ALU.subtract, [gb("rt")], [gb("rt")])
                                ACT(rt[:, 42:43], rt[:, 42:43], AF.Exp, [gb("rt")], [gb("rt")])
                                TS(rt[:, 43:44], rt[:, 42:43], 1.0, None, ALU.add, None, [gb("rt")], [gb("rt")])
                                RECIP(rt[:, 43:44], rt[:, 43:44], [gb("rt")], [gb("rt")])
                                TT(rt[:, 44:45], rt[:, 42:43], rt[:, 43:44], ALU.mult, [gb("rt")], [gb("rt")])
                                TS(rt[:, 32:40], rt[:, 8:16], rt[:, 43:44], None, ALU.mult, None, [gb("rt")], [gb("rt")])
                                STT(WT[:, gt, :], rt[:, 24:32], rt[:, 44:45], rt[:, 32:40], ALU.mult, ALU.add, [gb("rt")], [gb("WT")])
                    norm_mod(T, range(nblk), N, wm[:, 8:16], mc[:, 24:32], hT, XB, UB, f32_tap=tap)
                else:
                    norm_mod(T, range(nblk), N, wm[:, 8:16], mc[:, 24:32], hT, XB, UB)

                act = [PR[:, i * 2048:(i + 1) * 2048].rearrange("p (f n) -> p f n", f=4) for i in range(2)]
                experts = range(NEXP) if is_moe else [None]
                items = [(ex, gi, bi) for ex in experts for gi in range(7) for bi in range(nblk)]
                gstate = {}

                def GU(k, ex, gi, bi):
                    if bi == 0:
                        w13 = moe_w13[0, ex] if is_moe else ffn_w13[0]
                        w2 = moe_w2[0, ex] if is_moe else ffn_w2[0]
                        gidx = len(gstate)
                        Wg, Wu, W2 = ffw(gidx % 2)
                        fB = gb("ffw%d" % (gidx % 2))
                        DMAW(Wg, w13[:, gi * 512:(gi + 1) * 512].rearrange("(k p) n -> p k n", p=128), [], [fB])
                        DMAW(Wu, w13[:, FF + gi * 512:FF + (gi + 1) * 512].rearrange("(k p) n -> p k n", p=128), [], [fB])
                        DMAW(W2, w2[gi * 512:(gi + 1) * 512, :].rearrange("(f p) n -> p f n", p=128), [], [fB])
                        gstate[(ex, gi)] = (Wg, Wu, W2, fB)
                    Wg, Wu, W2, fB = gstate[(ex, gi)]
                    sl = slice(bi * N, (bi + 1) * N)
                    a_t = act[k % 2]
                    aB = gb("act%d" % (k % 2))
                    if is_moe:
                        gsrc = ft[2] if bi < 2 else ft[4]
                        Gt = gsrc[:, :].bitcast(BF16)[:, (bi % 2) * 512:(bi % 2) * 512 + N]
                        GtB = gb("gate%d" % bi)
                        if gi == 0:
                            pgb = bank()
                            for tt in range(ntt):
                                gt = bi * ntt + tt
                                TS(bt[2][:, tt * 128:(tt + 1) * 128], onesb[:], WT[:, gt, ex:ex + 1], None, ALU.mult, None, [gb("onesb"), gb("WT")], [gb("bt2")])
                            MM([(ps[pgb][:, tt * 128:(tt + 1) * 128], bt[2][:, tt * 128:(tt + 1) * 128], identb[:], True, True) for tt in range(ntt)],
                               [gb("bt2"), gb("identb")], [PB[pgb]])
                            ACT(Gt, ps[pgb][:, 0:N], AF.Copy, [PB[pgb]], [GtB])
                    for f in range(4):
                        pg = bank()
                        MM([(ps[pg][:, 0:N], Wg[:, kk, f * 128:(f + 1) * 128], hT[:, kk, sl], kk == 0, kk == 7) for kk in range(8)], [fB, UB[bi]], [PB[pg]])
                        pu = bank()
                        MM([(ps[pu][:, 0:N], Wu[:, kk, f * 128:(f + 1) * 128], hT[:, kk, sl], kk == 0, kk == 7) for kk in range(8)], [fB, UB[bi]], [PB[pu]])
                        sg = bt[f % 2]
                        sgB = gb("bt%d" % (f % 2))
                        ACT(sg[:, 0:N], ps[pg][:, 0:N], AF.Silu, [PB[pg]], [sgB])
                        if is_moe:
                            tmpf = ft[f % 2]
                            tfB = gb("ft%d" % (f % 2))
                            TT(tmpf[:, 0:N], ps[pu][:, 0:N], sg[:, 0:N], ALU.mult, [PB[pu], sgB], [tfB])
                            TT(a_t[:, f, 0:N], tmpf[:, 0:N], Gt, ALU.mult, [tfB, GtB], [aB])
                        else:
                            TT(a_t[:, f, 0:N], ps[pu][:, 0:N], sg[:, 0:N], ALU.mult, [PB[pu], sgB], [aB])

                def YY(k, ex, gi, bi):
                    Wg, Wu, W2, fB = gstate[(ex, gi)]
                    sl = slice(bi * N, (bi + 1) * N)
                    a_t = act[k % 2]
                    aB = gb("act%d" % (k % 2))
                    for c2 in range(8):
                        py = bank()
                        MM([(ps[py][:, 0:N], W2[:, f, c2 * 128:(c2 + 1) * 128], a_t[:, f, 0:N], f == 0, f == 3) for f in range(4)], [fB, aB], [PB[py]])
                        STT(xT[:, c2, sl], ps[py][:, 0:N], mc[:, 40 + c2:41 + c2], xT[:, c2, sl], ALU.mult, ALU.add, [PB[py], gb("modc"), XB[bi]], [XB[bi]])

                prev = None
                for k, (ex, gi, bi) in enumerate(items):
                    GU(k, ex, gi, bi)
                    if prev is not None:
                        YY(*prev)
                    prev = (k, ex, gi, bi)
                YY(*prev)
                S.barrier()
                ck("ffn")

            if is_ctx:
                return
            for bi in range(nblk):
                t0 = bi * N
                pb = bank()
                for c in range(8):
                    sq = bt[c % 2]
                    sqB = gb("bt%d" % (c % 2))
                    ACT(sq[:, 0:N], xT[:, c, t0:t0 + N], AF.Square, [XB[bi]], [sqB])
                    MM([(ps[pb][:, 0:N], onesb[:], sq[:, 0:N], c == 0, c == 7)], [sqB, gb("onesb")], [PB[pb]])
                rstd = ft[4]
                ACT(rstd[:, 0:N], ps[pb][:, 0:N], AF.Sqrt, [PB[pb], gb("cst")], [gb("ft4")], scale=1.0 / D, bias=cst[:, 0:1])
                RECIP(rstd[:, 0:N], rstd[:, 0:N], [gb("ft4")], [gb("ft4")])
                for c in range(8):
                    tmp = ft[c % 2]
                    tB = gb("ft%d" % (c % 2))
                    STT(tmp[:, 0:N], xT[:, c, t0:t0 + N], pcolB[:, 96 + c:97 + c], rstd[:, 0:N], ALU.mult, ALU.mult, [XB[bi], gb("ft4"), gb("pcol")], [tB])
                    pt = bank()
                    for tt in range(ntt):
                        TR(ps[pt][:, tt * 128:(tt + 1) * 128], tmp[:, tt * 128:(tt + 1) * 128], [tB], [PB[pt]])
                    if c % 2 == 0:
                        ACT(iost[:, 0:ntt, c * 128:(c + 1) * 128], ps[pt][:, 0:N].rearrange("p (t d) -> p t d", d=128), AF.Copy, [PB[pt]], [gb("iost")])
                    else:
                        COPYV(iost[:, 0:ntt, c * 128:(c + 1) * 128], ps[pt][:, 0:N].rearrange("p (t d) -> p t d", d=128), [PB[pt]], [gb("iost")])
                DMAS(out_d[b, t0:t0 + N, :].rearrange("(t p) d -> p t d", p=128), iost[:, 0:ntt, :], [gb("iost")], [gb("outd")])
            S.barrier()

        try:
            if stop != -1:
                for b in range(NB):
                    run_pass(b, True)
                    run_pass(b, False)
        except _Stop:
            S.barrier()
            dbg = []
            for c in range(8):
                dbg.append((uT[:, c, 0:256], c, 256))
                dbg.append((xT[:, c, 0:256], 20 + c, 256))
                dbg.append((xT[:, c, 1792:2048], 40 + c, 256))
            for j in range(4):
                dbg.append((yD[:, j, 0:256], 8 + j, 256))
                dbg.append((yA[:, j, 0:256], 12 + j, 256))
                dbg.append((yR[:, j, 0:256], 16 + j, 256))
                dbg.append((yD[:, j, 1792:2048], 32 + j, 256))
                dbg.append((yR[:, j, 1792:2048], 36 + j, 256))
                dbg.append((yA[:, j, 1792:2048], 53 + j, 256))
            dbg += [(kT[:, 0:256], 28, 256), (qT[:, 0:256], 29, 256), (qT2[:, 0:256], 30, 256),
                    (vtok[:, 0:2, :], 31, 256),
                    (modc[:].rearrange("p l r o -> p (l r o)"), 48, DEPTH * (NB + 1) * 48), (wm[:], 49, 16),
                    (pcolA[:].rearrange("p l o -> p (l o)"), 50, 192), (pcolB[:], 51, 112),
                    (hsave[:].rearrange("p l j d -> p (l j d)"), 52, 16), (WTt[:].rearrange("p t e -> p (t e)"), 57, 128),
                    (lamc[:].rearrange("p l o -> p (l o)"), 58, 8)]
            for ap, k, n in dbg:
                r0 = (k // 4) * 128
                c0 = (k % 4) * 256
                if len(ap.shape) == 3:
                    COPYV(ft[0][:, 0:n].rearrange("p (a b) -> p a b", a=ap.shape[1]), ap, [], [gb("ft0")])
                else:
                    COPYV(ft[0][:, 0:n], ap, [], [gb("ft0")])
                DMAS(out_d[0, r0:r0 + 128, c0:c0 + n], ft[0][:, 0:n], [gb("ft0")], [gb("outd")])
        S.finish("sp")
        with nc.Block() as block:
            S.replay(block)
    return nc


def _consts():
    ident = np.eye(128, dtype=np.float32)
    pm = np.zeros((128, 128), np.float32)
    for m in range(128):
        i = m % 32
        partner = m + 16 if i < 16 else m - 16
        pm[partner, m] = 1.0
    n = SEQ
    rows = n // 64
    pos_r = np.repeat(np.arange(rows, dtype=np.float32), 64)
    pos_c = np.tile(np.arange(64, dtype=np.float32), rows)
    inv_freq = (10000.0 ** (-np.arange(0, 32, 2, dtype=np.float32) / 32)).astype(np.float32)
    cos_t = np.zeros((128, n), np.float32)
    sin_t = np.zeros((128, n), np.float32)
    for m in range(128):
        axis = (m % 64) // 32
        half = (m % 32) // 16
        i = m % 16
        pos = pos_r if axis == 0 else pos_c
        ang = (pos * inv_freq[i]).astype(np.float32)
        cos_t[m] = np.cos(ang)
        sin_t[m] = np.sin(ang) * (-1.0 if half == 0 else 1.0)
    return ident, pm, np.stack([cos_t, sin_t]).astype(np.float32)


_NC_CACHE = {}


def run_batches(inputs, batch_lists, n_layers=DEPTH):
    NB = len(batch_lists[0])
    key = (NB, n_layers)
    if key not in _NC_CACHE:
        _NC_CACHE[key] = build_nc(NB, n_layers)
    nc = _NC_CACHE[key]
    ident, pm, rope = _consts()
    f = lambda a: np.ascontiguousarray(np.asarray(a, dtype=np.float32))
    shared = {k: f(inputs[k]) for k in (
        "mod_w", "mod_b", "norm1_w", "norm2_w", "w_in", "conv_a_w", "rg_conv_w", "rg_conv_b", "rg_wa", "rg_ba",
        "rg_wx", "rg_bx", "rg_lambda", "da_lambda", "da_subln_w", "w_branch", "w_out", "ffn_w13", "ffn_w2",
        "router_w", "moe_w13", "moe_w2", "final_norm_w")}
    shared["ident"] = ident
    shared["pmat"] = pm
    shared["rope"] = rope
    x = f(inputs["x"])
    ctx = f(inputs["ctx"])
    c = f(inputs["c"])
    c_ctx = f(inputs["c_ctx"])
    in_maps = []
    for bl in batch_lists:
        m = dict(shared)
        m["x"] = np.ascontiguousarray(x[bl])
        m["ctx"] = np.ascontiguousarray(ctx[bl])
        m["cc"] = np.ascontiguousarray(np.concatenate([c[bl], c_ctx[None, :]], axis=0))
        in_maps.append(m)
    res = run_bass_kernel_spmd(nc, in_maps, core_ids=list(range(len(batch_lists))))
    return [r["out"] for r in res.results]


def kernel(**inputs):
    nb = inputs["x"].shape[0] // NCORE
    bl = [list(range(i * nb, (i + 1) * nb)) for i in range(NCORE)]
    outs = run_batches(inputs, bl)
    return np.concatenate(outs, axis=0).astype(np.float32)
```
